# Optimizing a Trainium2 kernel written in Bass

```python
import math
import jax
import jax.numpy as jnp
from jax import lax
import numpy as np

D_MODEL = 2048
BATCH = 8
SEQ = 2048
DEPTH = 4

GRID_W = 64
CTX_LEN = 256
D_MIX = D_MODEL
ATT_HEADS = 8
ATT_HEAD_DIM = 128
ATT_WIDTH = ATT_HEADS * ATT_HEAD_DIM
WIN_R = 8
WIN_C = 16
ROPE_THETA = 10000.0
SSD_WIDTH = D_MIX - ATT_WIDTH
SSD_HEAD_DIM = 64
SSD_HEADS = SSD_WIDTH // SSD_HEAD_DIM
SSD_GROUPS = 2
SSD_STATE = 128
SSD_CONV = 5
SSD_CHUNK = 128
XBC_WIDTH = SSD_WIDTH + 2 * SSD_GROUPS * SSD_STATE
PROJ_WIDTH = 3 * ATT_WIDTH + SSD_WIDTH + XBC_WIDTH + 2 * SSD_HEADS
PEER_HEADS = 8
PEER_NKEYS = 128
PEER_N = PEER_NKEYS * PEER_NKEYS
PEER_QDIM = 256
PEER_TOPK = 16
PEER_BLOCK = 128
DEEPNORM_ALPHA = (2 * DEPTH) ** 0.25
DEEPNORM_BETA = (8 * DEPTH) ** -0.25
N_MOD = 6
EPS = 1e-6

kernel_name = 'hybrid_natten_ssd_peer_diffusion_trunk'


def _layer_norm(x, g, b):
    xf = x.astype(jnp.float32)
    mu = jnp.mean(xf, axis=-1, keepdims=True)
    var = jnp.mean(jnp.square(xf - mu), axis=-1, keepdims=True)
    return ((xf - mu) * lax.rsqrt(var + EPS)).astype(g.dtype) * g + b


def _rms_norm(x, g):
    xf = x.astype(jnp.float32)
    return (xf * lax.rsqrt(jnp.mean(xf * xf, axis=-1, keepdims=True) + EPS)).astype(g.dtype) * g


def _split_proj(p):
    cuts = [ATT_WIDTH, 2 * ATT_WIDTH, 3 * ATT_WIDTH, 3 * ATT_WIDTH + SSD_WIDTH,
            3 * ATT_WIDTH + SSD_WIDTH + XBC_WIDTH]
    return jnp.split(p, cuts, axis=-1)


def _heads(t):
    return t.reshape(t.shape[:2] + (ATT_HEADS, ATT_HEAD_DIM))


def _axial_rope(x):
    L = x.shape[1]
    t = jnp.arange(L)
    pos = jnp.stack([t // GRID_W, t % GRID_W], axis=-1).astype(jnp.float32)
    axis_dim = ATT_HEAD_DIM // 2
    inv_freq = ROPE_THETA ** (-jnp.arange(0, axis_dim, 2, dtype=jnp.float32) / axis_dim)
    ang = pos[:, :, None] * inv_freq
    cos = jnp.cos(ang)[None, :, None]
    sin = jnp.sin(ang)[None, :, None]
    xr = x.reshape(x.shape[:-1] + (2, 2, axis_dim // 2))
    x1, x2 = xr[..., 0, :], xr[..., 1, :]
    out = jnp.stack([x1 * cos - x2 * sin, x1 * sin + x2 * cos], axis=-2)
    return out.reshape(x.shape).astype(x.dtype)


def _neighbourhood_attention(q, k, v, k_ctx, v_ctx, rpb):
    Bt, L, H, dh = q.shape
    rows = L // GRID_W
    wr = min(WIN_R, rows)
    r_idx = jnp.arange(rows)
    c_idx = jnp.arange(GRID_W)
    row_start = jnp.clip(r_idx - WIN_R // 2, 0, rows - wr)
    key_rows = row_start[:, None] + jnp.arange(wr)[None, :]
    col_start = jnp.clip(c_idx - WIN_C // 2, 0, GRID_W - WIN_C)
    col_ok = (c_idx[None, :] >= col_start[:, None]) & (c_idx[None, :] < col_start[:, None] + WIN_C)
    qg = q.reshape(Bt, rows, GRID_W, H, dh)
    kg = k.reshape(Bt, rows, GRID_W, H, dh)[:, key_rows]
    vg = v.reshape(Bt, rows, GRID_W, H, dh)[:, key_rows]
    dr = key_rows - r_idx[:, None] + (WIN_R - 1)
    dc = jnp.clip(c_idx[None, :] - c_idx[:, None] + (WIN_C - 1), 0, 2 * WIN_C - 2)
    bias = rpb[:, dr[:, None, :, None], dc[None, :, None, :]]
    s_nb = jnp.einsum('brqhd,brkwhd->bhrqkw', qg, kg).astype(jnp.float32) + bias
    s_nb = jnp.where(col_ok[:, None, :], s_nb, -jnp.inf)
    s_ctx = jnp.einsum('brqhd,bkhd->bhrqk', qg, k_ctx).astype(jnp.float32)
    n_nb = wr * GRID_W
    s = jnp.concatenate([s_nb.reshape(Bt, H, rows, GRID_W, n_nb), s_ctx], axis=-1)
    p = jax.nn.softmax(s, axis=-1).astype(v.dtype)
    p_nb = p[..., :n_nb].reshape(Bt, H, rows, GRID_W, wr, GRID_W)
    out = (jnp.einsum('bhrqkw,brkwhd->brqhd', p_nb, vg)
           + jnp.einsum('bhrqk,bkhd->brqhd', p[..., n_nb:], v_ctx))
    return out.reshape(Bt, L, H, dh)


def _context_attention(q, k, v):
    s = jnp.einsum('bqhd,bkhd->bhqk', q, k).astype(jnp.float32)
    p = jax.nn.softmax(s, axis=-1).astype(v.dtype)
    return jnp.einsum('bhqk,bkhd->bqhd', p, v)


def _dwconv(x, w, b):
    out = lax.conv_general_dilated(
        x, w[:, None, :], window_strides=(1,),
        padding=[(SSD_CONV // 2, SSD_CONV // 2)],
        dimension_numbers=('NWC', 'WIO', 'NWC'),
        feature_group_count=x.shape[-1])
    return out + b


def _ssd_inputs(xbc, dt_raw, conv_w, conv_b, dt_bias):
    xbc = jax.nn.silu(_dwconv(xbc, conv_w, conv_b))
    xs, bm, cm = jnp.split(xbc, [SSD_WIDTH, SSD_WIDTH + SSD_GROUPS * SSD_STATE], axis=-1)
    Bt, L = xs.shape[:2]
    rep = SSD_HEADS // SSD_GROUPS
    xs = xs.reshape(Bt, L, SSD_HEADS, SSD_HEAD_DIM)
    bm = jnp.repeat(bm.reshape(Bt, L, SSD_GROUPS, SSD_STATE), rep, axis=2)
    cm = jnp.repeat(cm.reshape(Bt, L, SSD_GROUPS, SSD_STATE), rep, axis=2)
    dt = jax.nn.softplus((dt_raw.reshape(Bt, L, 2, SSD_HEADS) + dt_bias).astype(jnp.float32))
    return xs, bm, cm, dt


def _segsum(a):
    T = a.shape[-1]
    a_rep = jnp.broadcast_to(a[..., :, None], a.shape + (T,))
    a_rep = jnp.where(jnp.tril(jnp.ones((T, T), dtype=bool), -1), a_rep, 0.0)
    seg = jnp.cumsum(a_rep, axis=-2)
    return jnp.where(jnp.tril(jnp.ones((T, T), dtype=bool)), seg, -jnp.inf)


def _ssd_scan(x, dt, a, bm, cm, h0, with_y):
    Bt, L, H, P = x.shape
    N = bm.shape[-1]
    nc = L // SSD_CHUNK
    xd = (x * dt[..., None]).reshape(Bt, nc, SSD_CHUNK, H, P)
    bm = bm.reshape(Bt, nc, SSD_CHUNK, H, N)
    cm = cm.reshape(Bt, nc, SSD_CHUNK, H, N)
    a_dt = (dt * a).reshape(Bt, nc, SSD_CHUNK, H).transpose(0, 3, 1, 2)
    a_cs = jnp.cumsum(a_dt, axis=-1)
    decay_states = jnp.exp(a_cs[..., -1:] - a_cs)
    states = jnp.einsum('bclhn,bhcl,bclhp->bchpn', bm, decay_states, xd)
    states = jnp.concatenate([h0[:, None].astype(states.dtype), states], axis=1)
    decay_chunk = jnp.exp(_segsum(jnp.pad(a_cs[..., -1], ((0, 0), (0, 0), (1, 0)))))
    states = jnp.einsum('bhzc,bchpn->bzhpn', decay_chunk, states)
    final = states[:, -1]
    if not with_y:
        return None, final
    l_mat = jnp.exp(_segsum(a_dt))
    y_diag = jnp.einsum('bclhn,bcshn,bhcls,bcshp->bclhp', cm, bm, l_mat, xd)
    y_off = jnp.einsum('bclhn,bchpn,bhcl->bclhp', cm, states[:, :-1], jnp.exp(a_cs))
    return (y_diag + y_off).reshape(Bt, L, H, P).astype(x.dtype), final


def _bidirectional_ssd(ssd_c, ssd_l, a_log, d_skip, with_ctx):
    xs_c, bm_c, cm_c, dt_c = ssd_c
    xs_l, bm_l, cm_l, dt_l = ssd_l
    a = -jnp.exp(a_log.astype(jnp.float32))
    h_zero = jnp.zeros((xs_c.shape[0], SSD_HEADS, SSD_HEAD_DIM, SSD_STATE), jnp.float32)
    ys_c, ys_l = [], []
    for d in range(2):
        f = (lambda t: jnp.flip(t, axis=1)) if d == 1 else (lambda t: t)
        yc, h_ctx = _ssd_scan(f(xs_c), f(dt_c[:, :, d]), a[d], f(bm_c), f(cm_c), h_zero, with_ctx)
        yl, _ = _ssd_scan(f(xs_l), f(dt_l[:, :, d]), a[d], f(bm_l), f(cm_l), h_ctx, True)
        ys_l.append(f(yl) + d_skip[d][:, None] * xs_l)
        if with_ctx:
            ys_c.append(f(yc) + d_skip[d][:, None] * xs_c)
    y_c = ys_c[0] + ys_c[1] if with_ctx else None
    return y_c, ys_l[0] + ys_l[1]


def _mixer(h_c, h_l, w_in, conv_w, conv_b, a_log, dt_bias, d_skip, rpb,
           beta_attn, beta_ssm, w_out, with_ctx):
    q_c, k_c, v_c, z_c, xbc_c, dt_c = _split_proj(h_c @ w_in)
    q_l, k_l, v_l, z_l, xbc_l, dt_l = _split_proj(h_l @ w_in)
    scale = ATT_HEAD_DIM ** -0.5
    k_c, v_c = _heads(k_c), _heads(v_c)
    att_l = _neighbourhood_attention(_axial_rope(_heads(q_l)) * scale, _axial_rope(_heads(k_l)),
                                     _heads(v_l), k_c, v_c, rpb)
    ssd_c = _ssd_inputs(xbc_c, dt_c, conv_w, conv_b, dt_bias)
    ssd_l = _ssd_inputs(xbc_l, dt_l, conv_w, conv_b, dt_bias)
    y_c, y_l = _bidirectional_ssd(ssd_c, ssd_l, a_log, d_skip, with_ctx)

    def merge(att, y, z):
        att = _rms_norm(att.reshape(z.shape[:2] + (ATT_WIDTH,)), beta_attn)
        ssm = _rms_norm(y.reshape(z.shape[:2] + (SSD_WIDTH,)) * jax.nn.silu(z), beta_ssm)
        return jnp.concatenate([att, ssm], axis=-1) @ w_out

    out_l = merge(att_l, y_l, z_l)
    if not with_ctx:
        return None, out_l
    att_c = _context_attention(_heads(q_c) * scale, k_c, v_c)
    return merge(att_c, y_c, z_c), out_l


def _peer(h, wq, sub_keys, u_tab, v_tab):
    T = h.shape[0]
    q = (h @ wq).reshape(T, PEER_HEADS, 2, PEER_QDIM // 2)
    s = jnp.einsum('thad,hakd->thak', q, sub_keys).astype(jnp.float32)
    s_top, i_top = lax.top_k(s, PEER_TOPK)
    kk = PEER_TOPK * PEER_TOPK
    cand_s = (s_top[..., 0, :, None] + s_top[..., 1, None, :]).reshape(T, PEER_HEADS, kk)
    cand_i = (i_top[..., 0, :, None] * PEER_NKEYS + i_top[..., 1, None, :]).reshape(T, PEER_HEADS, kk)
    best_s, best_pos = lax.top_k(cand_s, PEER_TOPK)
    idx = jnp.take_along_axis(cand_i, best_pos, axis=-1)
    gate = jax.nn.softmax(best_s, axis=-1).astype(h.dtype)
    nb = T // PEER_BLOCK

    def block(args):
        hb, ib, gb = args
        act = jax.nn.gelu(jnp.einsum('td,thkd->thk', hb, u_tab[ib]))
        return jnp.einsum('thk,thkd->td', gb * act, v_tab[ib])

    out = lax.map(block, (h.reshape(nb, PEER_BLOCK, D_MODEL),
                          idx.reshape(nb, PEER_BLOCK, PEER_HEADS, PEER_TOPK),
                          gate.reshape(nb, PEER_BLOCK, PEER_HEADS, PEER_TOPK)))
    return out.reshape(T, D_MODEL)


def setup_inputs(seed: int = 0) -> dict:
    key = jax.random.key(seed)
    ks = jax.random.split(key, 24)

    def nrm(k, shape, s):
        return jax.random.normal(k, shape, jnp.float32) * s

    a_log = jnp.log(jax.random.uniform(ks[9], (DEPTH, 2, SSD_HEADS), jnp.float32, 1.0, 16.0))
    dt0 = jnp.exp(jax.random.uniform(ks[10], (DEPTH, 2, SSD_HEADS), jnp.float32,
                                     math.log(1e-3), math.log(1e-1)))
    dt_bias = dt0 + jnp.log(-jnp.expm1(-dt0))
    return {
        'x': nrm(ks[0], (BATCH, SEQ, D_MODEL), 1.0),
        'c': nrm(ks[1], (BATCH, D_MODEL), 1.0),
        'ctx': nrm(ks[2], (BATCH, CTX_LEN, D_MODEL), 1.0),
        'c_ctx': nrm(ks[3], (D_MODEL,), 1.0),
        'ada_w': nrm(ks[4], (DEPTH, D_MODEL, N_MOD * D_MODEL), 0.5 * D_MODEL ** -0.5),
        'ada_b': nrm(ks[5], (DEPTH, N_MOD * D_MODEL), 0.02),
        'w_in': nrm(ks[6], (DEPTH, D_MODEL, PROJ_WIDTH), D_MODEL ** -0.5),
        'conv_w': nrm(ks[7], (DEPTH, SSD_CONV, XBC_WIDTH), SSD_CONV ** -0.5),
        'conv_b': nrm(ks[8], (DEPTH, XBC_WIDTH), 0.02),
        'a_log': a_log,
        'dt_bias': dt_bias,
        'd_skip': 1.0 + nrm(ks[11], (DEPTH, 2, SSD_HEADS), 0.02),
        'rpb': nrm(ks[12], (DEPTH, ATT_HEADS, 2 * WIN_R - 1, 2 * WIN_C - 1), 0.02),
        'beta_attn': 1.0 + nrm(ks[13], (DEPTH, ATT_WIDTH), 0.02),
        'beta_ssm': 1.0 + nrm(ks[14], (DEPTH, SSD_WIDTH), 0.02),
        'w_out': nrm(ks[15], (DEPTH, D_MIX, D_MODEL), DEEPNORM_BETA * D_MIX ** -0.5),
        'ln1_g': 1.0 + nrm(ks[16], (DEPTH, D_MODEL), 0.02),
        'ln1_b': nrm(ks[17], (DEPTH, D_MODEL), 0.02),
        'peer_wq': nrm(ks[18], (DEPTH, D_MODEL, PEER_HEADS * PEER_QDIM), D_MODEL ** -0.5),
        'peer_keys': nrm(ks[19], (DEPTH, PEER_HEADS, 2, PEER_NKEYS, PEER_QDIM // 2), (PEER_QDIM // 2) ** -0.5),
        'peer_u': nrm(ks[20], (DEPTH, PEER_N, D_MODEL), D_MODEL ** -0.5),
        'peer_v': nrm(ks[21], (DEPTH, PEER_N, D_MODEL), DEEPNORM_BETA),
        'ln2_g': 1.0 + nrm(ks[22], (DEPTH, D_MODEL), 0.02),
        'ln2_b': nrm(ks[23], (DEPTH, D_MODEL), 0.02),
    }


def reference(x, c, ctx, c_ctx, ada_w, ada_b, w_in, conv_w, conv_b, a_log, dt_bias,
              d_skip, rpb, beta_attn, beta_ssm, w_out, ln1_g, ln1_b, peer_wq,
              peer_keys, peer_u, peer_v, ln2_g, ln2_b):
    xc = ctx
    silu_c = jax.nn.silu(c)
    silu_cc = jax.nn.silu(c_ctx)
    for layer in range(DEPTH):
        with_ctx = layer < DEPTH - 1
        sh1, sc1, g1, sh2, sc2, g2 = jnp.split(
            (silu_c @ ada_w[layer] + ada_b[layer])[:, None, :], N_MOD, axis=-1)
        csh1, csc1, cg1, csh2, csc2, cg2 = jnp.split(
            silu_cc @ ada_w[layer] + ada_b[layer], N_MOD, axis=-1)
        m_c, m_l = _mixer(xc * (1.0 + csc1) + csh1, x * (1.0 + sc1) + sh1,
                          w_in[layer], conv_w[layer], conv_b[layer], a_log[layer],
                          dt_bias[layer], d_skip[layer], rpb[layer], beta_attn[layer],
                          beta_ssm[layer], w_out[layer], with_ctx)
        x = _layer_norm(DEEPNORM_ALPHA * x + g1 * m_l, ln1_g[layer], ln1_b[layer])
        h_l = x * (1.0 + sc2) + sh2
        if with_ctx:
            xc = _layer_norm(DEEPNORM_ALPHA * xc + cg1 * m_c, ln1_g[layer], ln1_b[layer])
            h_c = xc * (1.0 + csc2) + csh2
            n_c = h_c.shape[0] * h_c.shape[1]
            f_all = _peer(jnp.concatenate([h_c.reshape(-1, D_MODEL), h_l.reshape(-1, D_MODEL)], axis=0),
                          peer_wq[layer], peer_keys[layer], peer_u[layer], peer_v[layer])
            f_c = f_all[:n_c].reshape(h_c.shape)
            f_l = f_all[n_c:].reshape(h_l.shape)
            xc = _layer_norm(DEEPNORM_ALPHA * xc + cg2 * f_c, ln2_g[layer], ln2_b[layer])
        else:
            f_l = _peer(h_l.reshape(-1, D_MODEL), peer_wq[layer], peer_keys[layer],
                        peer_u[layer], peer_v[layer]).reshape(h_l.shape)
        x = _layer_norm(DEEPNORM_ALPHA * x + g2 * f_l, ln2_g[layer], ln2_b[layer])
    return x
```

```python
import os
import re
import numpy as np
from contextlib import ExitStack
import concourse.bass as bass
import concourse.mybir as mybir
from concourse.bass_utils import run_bass_kernel_spmd

F32 = mybir.dt.float32
BF16 = mybir.dt.bfloat16
AF = mybir.ActivationFunctionType
ALU = mybir.AluOpType
AX = mybir.AxisListType

D = 2048
TC = 256
TL = 2048
T = TC + TL
NT = T // 128
PROJ = 5664
DEPTH = 4
ALPHA = (2 * DEPTH) ** 0.25
EPS = 1e-6
NEG = -30000.0
PADW = 2312
TBLK = [(0, 256), (256, 512), (768, 512), (1280, 512), (1792, 512)]


def tokcol(t):
    return 2 + t if t < TC else 262 + (t - TC)


class Res:
    __slots__ = ("name", "writer", "readers")

    def __init__(self, name):
        self.name = name
        self.writer = None
        self.readers = []


class Prog:
    NDMA = 24

    def __init__(self, nc, stack):
        self.nc = nc
        self.eng = {"pe": nc.tensor, "dve": nc.vector, "act": nc.scalar,
                    "pool": nc.gpsimd, "sp": nc.sync}
        self.sems = {}
        self.count = {}
        for k in list(self.eng) + ["d%d" % i for i in range(self.NDMA)]:
            self.sems[k] = stack.enter_context(nc.semaphore("s_" + k))
            self.count[k] = 0
        self.seen = {e: {} for e in self.eng}
        self.dma_rr = 0
        self.ninst = 0
        self.uid = 0

    def _wait(self, e, k, v):
        if self.seen[e].get(k, 0) >= v:
            return
        self.eng[e].wait_ge(self.sems[k], v)
        self.seen[e][k] = v
        self.ninst += 1

    _PS = re.compile(r"^(pc|lvp|pmod|pg|ptr\d|pa\d|pb\d|ptp|psA\d|psB\d|po\d|pcs|pD\d|pG|pY|pm\d|pt32|pr\w*)$")

    def _deps(self, reads, writes):
        deps = {}
        for r in reads:
            if r.writer is not None:
                k, v = r.writer
                if deps.get(k, 0) < v:
                    deps[k] = v
            if self._PS.match(r.name):
                for k, v in r.readers:
                    if deps.get(k, 0) < v:
                        deps[k] = v
        for w in writes:
            if w.writer is not None:
                k, v = w.writer
                if deps.get(k, 0) < v:
                    deps[k] = v
            for k, v in w.readers:
                if deps.get(k, 0) < v:
                    deps[k] = v
        return deps

    @staticmethod
    def _record(key, val, reads, writes):
        for r in reads:
            r.readers.append((key, val))
            if len(r.readers) > 48:
                m = {}
                for k, v in r.readers:
                    if m.get(k, 0) < v:
                        m[k] = v
                r.readers = list(m.items())
        for w in writes:
            w.writer = (key, val)
            w.readers = []

    def op(self, e, fn, reads=(), writes=()):
        deps = self._deps(reads, writes)
        for k, v in deps.items():
            if e == "pe" and k == "pe":
                continue
            self._wait(e, k, v)
        ins = fn(self.eng[e])
        self.count[e] += 1
        ins.then_inc(self.sems[e], 1)
        self.ninst += 1
        self._record(e, self.count[e], reads, writes)
        return ins

    def dma(self, out, in_, reads=(), writes=(), e="sp", **kw):
        k = "d%d" % self.dma_rr
        self.dma_rr = (self.dma_rr + 1) % self.NDMA
        deps = self._deps(reads, writes)
        if self.count[k] > 0 and deps.get(k, 0) < self.count[k]:
            deps[k] = self.count[k]
        for kk, v in deps.items():
            self._wait(e, kk, v)
        ins = self.eng[e].dma_start(out=out, in_=in_, **kw)
        self.count[k] += 16
        ins.then_inc(self.sems[k], 16)
        self.ninst += 1
        self._record(k, self.count[k], reads, writes)
        return ins

    def barrier(self, engines=("pe", "dve", "act", "pool", "sp")):
        for e in engines:
            for k in self.count:
                if self.count[k] > 0:
                    self._wait(e, k, self.count[k])


def host_consts():
    c = {}
    c["ident"] = np.eye(128, dtype=np.float32)
    perm = np.zeros((128, 128), np.float32)
    for i in range(128):
        a, r = divmod(i, 64)
        p = a * 64 + (r + 32) % 64
        perm[p, i] = 1.0
    c["perm"] = perm
    t = np.arange(TL)
    pos = np.stack([t // 64, t % 64], -1).astype(np.float32)
    inv = (10000.0 ** (-np.arange(0, 64, 2, dtype=np.float32) / 64)).astype(np.float32)
    ang = pos[:, :, None] * inv
    cos = np.ones((128, T), np.float32)
    sin = np.zeros((128, T), np.float32)
    for a in range(2):
        cos[a * 64:a * 64 + 32, TC:] = np.cos(ang[:, a, :]).T
        cos[a * 64 + 32:a * 64 + 64, TC:] = np.cos(ang[:, a, :]).T
        sin[a * 64:a * 64 + 32, TC:] = -np.sin(ang[:, a, :]).T
        sin[a * 64 + 32:a * 64 + 64, TC:] = np.sin(ang[:, a, :]).T
    c["cos"] = cos
    c["sin"] = sin
    k = np.arange(128)[:, None]
    l = np.arange(128)[None, :]
    c["m_le"] = (k <= l).astype(np.float32)
    c["m_ge"] = (k >= l).astype(np.float32)
    c["m_gt"] = (k > l).astype(np.float32)
    c["m_lt"] = (k < l).astype(np.float32)
    c["ones"] = np.ones((128, 128), np.float32)
    return c


VAR_I = [0, 1, 7, 14, 15]


def pair_variant(i):
    return {0: 0, 1: 1, 14: 3, 15: 4}.get(i, 2)


def pair_c0(i):
    return min(max(i - 2, 0), 11)


def host_bias(rpb):
    L = rpb.shape[0]
    out = np.empty((L, 8, 5, 128, 5, 128), np.float32)
    kl = np.arange(128)[:, None, None]
    ch = np.arange(5)[None, :, None]
    ql = np.arange(128)[None, None, :]
    for v, i in enumerate(VAR_I):
        c0 = pair_c0(i)
        qr = 2 * i + ql // 64
        qc = ql % 64
        kr = 2 * (c0 + ch) + kl // 64
        kc = kl % 64
        rs = np.clip(qr - 4, 0, 24)
        cs = np.clip(qc - 8, 0, 48)
        valid = (kr >= rs) & (kr < rs + 8) & (kc >= cs) & (kc < cs + 16)
        dr = np.clip(kr - qr + 7, 0, 14)
        dc = np.clip(kc - qc + 15, 0, 30)
        dr, dc, valid = np.broadcast_arrays(dr, dc, valid)
        g = rpb[:, :, dr, dc]
        out[:, :, v] = np.where(valid[None, None], g, np.float32(NEG))
    return out


class Builder:
    def __init__(self, depth=DEPTH, dbg=(), stop=None):
        self.depth = depth
        self.dbg = set(dbg)
        self.stop = stop
        self.nc = bass.Bass("TRN2", target_bir_lowering=False)
        self.uid = 0

    def din(self, name, shape, dt=F32):
        return self.nc.dram_tensor(name, list(shape), dt, kind="ExternalInput").ap()

    def dscr(self, name, shape, dt=F32):
        kind = "ExternalOutput" if name in self.dbg else "Internal"
        return self.nc.dram_tensor(name, list(shape), dt, kind=kind).ap()

    def sb(self, st, name, shape, dt=F32):
        self.uid += 1
        return st.enter_context(self.nc.sbuf_tensor("%s_%d" % (name, self.uid), list(shape), dt))

    def ps(self, st, name, shape, dt=F32):
        self.uid += 1
        return st.enter_context(self.nc.psum_tensor("%s_%d" % (name, self.uid), list(shape), dt))

    def build(self):
        nc = self.nc
        dp = self.depth
        I = {}
        I["x"] = self.din("x", [TL, D])
        I["ctx"] = self.din("ctx", [TC, D])
        I["c2"] = self.din("c2", [32, 128])
        I["ada_w"] = self.din("ada_w", [dp, D, 6 * D])
        I["ada_b"] = self.din("ada_b", [dp, 96, 128])
        I["w_in"] = self.din("w_in", [dp, D, PROJ])
        I["conv_w"] = self.din("conv_w", [dp, 60, 128])
        I["conv_b"] = self.din("conv_b", [dp, 12, 128])
        I["a_log"] = self.din("a_log", [dp, 32])
        I["dt_bias"] = self.din("dt_bias", [dp, 32])
        I["d_skip"] = self.din("d_skip", [dp, 32])
        I["biasT"] = self.din("biasT", [dp, 8, 5, 128, 5, 128])
        I["beta"] = self.din("beta", [dp, D])
        I["w_out"] = self.din("w_out", [dp, D, D])
        for n in ("ln1_g", "ln1_b", "ln2_g", "ln2_b"):
            I[n] = self.din(n, [dp, D])
        I["peer_wq"] = self.din("peer_wq", [dp, D, D])
        I["peer_keys"] = self.din("peer_keys", [dp, 16, 128, 128])
        ne = 128 if (self.stop is not None and self.stop != "peer") else 16384
        I["peer_u"] = self.din("peer_u", [dp, ne, D])
        I["peer_v"] = self.din("peer_v", [dp, ne, D])
        for n in ("ident", "perm", "m_le", "m_ge", "m_gt", "m_lt", "ones"):
            I[n] = self.din(n, [128, 128])
        I["cos"] = self.din("cos", [128, T])
        I["sin"] = self.din("sin", [128, T])
        self.I = I
        self.out = nc.dram_tensor("out", [TL, D], F32, kind="ExternalOutput").ap()

        S = {}
        S["xres"] = self.dscr("xres", [T, D])
        S["gv"] = self.dscr("gv", [4, D])
        S["qT"] = self.dscr("qT", [8, 128, T], BF16)
        S["kT"] = self.dscr("kT", [8, 128, T], BF16)
        S["vaug"] = self.dscr("vaug", [NT, 128, 8, 129], BF16)
        S["zs"] = self.dscr("zs", [T, 1024], BF16)
        S["xs"] = self.dscr("xs", [T, 1024], BF16)
        S["btm"] = self.dscr("btm", [T, 256], BF16)
        S["bT"] = self.dscr("bT", [2, 128, T], BF16)
        S["cT"] = self.dscr("cT", [2, 128, T], BF16)
        S["att"] = self.dscr("att", [T, 1024])
        S["yf"] = self.dscr("yf", [T, 1024])
        S["ysz"] = self.dscr("ysz", [T, 1024])
        S["h2T"] = self.dscr("h2T", [16, 128, T], BF16)
        S["gT"] = self.dscr("gT", [128, 128, T], BF16)
        S["dtd"] = self.dscr("dtd", [T, 32])
        self.S = S
        self.RS = {k: Res(k) for k in S}

        with ExitStack() as st:
            self.P = P = Prog(nc, st)
            self.st = st
            C = {}
            self.RC = Res("consts")
            for n in ("ident", "perm", "m_le", "m_ge", "m_gt", "m_lt", "ones"):
                C[n] = self.sb(st, n, [128, 128])
                P.dma(C[n][:], I[n], writes=[self.RC])
            C["identb"] = self.sb(st, "identb", [128, 128], BF16)
            C["permb"] = self.sb(st, "permb", [128, 128], BF16)
            C["m_leb"] = self.sb(st, "m_leb", [128, 128], BF16)
            C["m_geb"] = self.sb(st, "m_geb", [128, 128], BF16)
            P.op("dve", lambda e: e.tensor_copy(C["identb"][:], C["ident"][:]), reads=[self.RC], writes=[self.RC])
            P.op("dve", lambda e: e.tensor_copy(C["permb"][:], C["perm"][:]), reads=[self.RC], writes=[self.RC])
            P.op("dve", lambda e: e.tensor_copy(C["m_leb"][:], C["m_le"][:]), reads=[self.RC], writes=[self.RC])
            P.op("dve", lambda e: e.tensor_copy(C["m_geb"][:], C["m_ge"][:]), reads=[self.RC], writes=[self.RC])
            self.C = C
            P.dma(S["xres"][0:TC, :], I["ctx"], writes=[self.RS["xres"]])
            for i in range(4):
                P.dma(S["xres"][TC + i * 512:TC + (i + 1) * 512, :], I["x"][i * 512:(i + 1) * 512, :],
                      writes=[self.RS["xres"]])
            self.scT = self.sb(st, "scT", [128, 16, 2])
            self.Rsc = Res("scT")
            with ExitStack() as ph:
                c2 = self.sb(ph, "c2", [32, 128])
                pc = self.ps(ph, "pc", [128, 32])
                r1, r2 = Res("c2"), Res("pc")
                P.dma(c2[:], I["c2"], writes=[r1])
                P.op("pe", lambda e: e.transpose(pc[:], c2[:], C["ident"][0:32, 0:32]), reads=[r1, self.RC], writes=[r2])
                for r in range(2):
                    P.op("act", lambda e, r=r: e.activation(out=self.scT[:, :, r], in_=pc[:, r * 16:(r + 1) * 16], func=AF.Silu),
                         reads=[r2], writes=[self.Rsc])
                P.barrier()
            P.barrier()
            for l in range(dp):
                self.layer(l)
                if self.stop is not None and l == 0:
                    break
            P.barrier()
            if self.stop is None:
                for i in range(4):
                    P.dma(self.out[i * 512:(i + 1) * 512, :], S["xres"][TC + i * 512:TC + (i + 1) * 512, :],
                          reads=[self.RS["xres"]])
            P.barrier(engines=("sp",))
        return nc

    def load_vecT(self, ph, dst, src_rows, n, rdst):
        P, C = self.P, self.C
        tmp = self.sb(ph, "lv", [n, 128])
        pt = self.ps(ph, "lvp", [128, n])
        r1, r2 = Res("lv"), Res("lvp")
        P.dma(tmp[:], src_rows, writes=[r1])
        P.op("pe", lambda e: e.transpose(pt[:], tmp[:], C["ident"][0:n, 0:n]), reads=[r1, self.RC], writes=[r2])
        P.op("dve", lambda e: e.tensor_copy(dst, pt[:]), reads=[r2], writes=[rdst])

    def layer(self, l):
        P = self.P
        with_ctx = l < DEPTH - 1 if self.depth == DEPTH else (l < self.depth - 1 or self.depth == 1)
        self.with_ctx = with_ctx
        with ExitStack() as ly:
            self.modT = self.sb(ly, "modT", [128, 96, 2])
            self.Rmod = Res("modT")
            self.phase_mod(l)
            P.barrier()
            if self.stop == "mod":
                return
            with ExitStack() as l2:
                self.hT = self.sb(l2, "hT", [128, 16, T], BF16)
                self.RhT = Res("hT")
                self.phase_hT()
                P.barrier()
                if self.stop == "hT":
                    return
                self.phase_proj(l)
                P.barrier()
            if self.stop == "proj":
                return
            self.phase_attn(l)
            P.barrier()
            if self.stop == "attn":
                return
            self.phase_ssd(l)
            P.barrier()
            if self.stop == "ssd":
                return
            self.phase_merge(l)
            P.barrier()
            if self.stop == "merge":
                return
        with ExitStack() as ly:
            self.phase_route(l)
            P.barrier()
            if self.stop == "route":
                return
            self.phase_peer(l)
            P.barrier()

    def phase_mod(self, l):
        P, C, I, S = self.P, self.C, self.I, self.S
        with ExitStack() as ph:
            wt = [self.sb(ph, "adaw", [128, 16, 512]) for _ in range(2)]
            rw = [Res("adaw0"), Res("adaw1")]
            pm = self.ps(ph, "pmod", [128, 192])
            rpm = Res("pmod")
            abT = self.sb(ph, "abT", [128, 96])
            rab = Res("abT")
            self.load_vecT(ph, abT[:], I["ada_b"][l], 96, rab)
            wv = I["ada_w"][l].rearrange("(k p) n -> p k n", p=128)
            for cg in range(24):
                b = cg % 2
                P.dma(wt[b][:], wv[:, :, cg * 512:(cg + 1) * 512], writes=[rw[b]])
                for jj in range(4):
                    j = cg * 4 + jj
                    for k in range(16):
                        P.op("pe", lambda e, b=b, jj=jj, j=j, k=k: e.matmul(
                            pm[:, 2 * j:2 * j + 2], wt[b][:, k, jj * 128:(jj + 1) * 128], self.scT[:, k, :],
                            start=(k == 0), stop=(k == 15)), reads=[rw[b], self.Rsc], writes=[rpm])
            P.op("dve", lambda e: e.tensor_tensor(out=self.modT[:], in0=pm[:].rearrange("p (j r) -> p j r", r=2),
                                                  in1=abT[:].unsqueeze(2).to_broadcast([128, 96, 2]), op=ALU.add),
                 reads=[rpm, rab], writes=[self.Rmod])
            for j0 in (16, 64):
                P.op("dve", lambda e, j0=j0: e.tensor_scalar_add(self.modT[:, j0:j0 + 16, :], self.modT[:, j0:j0 + 16, :], 1.0),
                     reads=[self.Rmod], writes=[self.Rmod])
            pg = self.ps(ph, "pg", [16, 4, 128])
            gsb = self.sb(ph, "gsb", [16, 4, 128])
            rpg, rgs = Res("pg"), Res("gsb")
            for gi, (j0, r) in enumerate([(32, 0), (32, 1), (80, 0), (80, 1)]):
                P.op("pe", lambda e, gi=gi, j0=j0, r=r: e.transpose(pg[:, gi, :], self.modT[:, j0:j0 + 16, r], C["ident"][:]),
                     reads=[self.Rmod, self.RC], writes=[rpg])
            P.op("dve", lambda e: e.tensor_copy(gsb[:], pg[:]), reads=[rpg], writes=[rgs])
            P.dma(S["gv"].rearrange("g (k p) -> k g p", p=128), gsb[:], reads=[rgs], writes=[self.RS["gv"]])
            P.barrier()

    def phase_hT(self):
        P, C, S = self.P, self.C, self.S
        with ExitStack() as ph:
            xt = [self.sb(ph, "xt", [128, D]) for _ in range(2)]
            rx = [Res("xt0"), Res("xt1")]
            pt = [self.ps(ph, "ptr", [128, 512]) for _ in range(2)]
            rp = [Res("ptr0"), Res("ptr1")]
            g = 0
            for tt in range(NT):
                b = tt % 2
                r = 1 if tt < 2 else 0
                P.dma(xt[b][:], S["xres"][tt * 128:(tt + 1) * 128, :], reads=[self.RS["xres"]], writes=[rx[b]])
                for kg in range(4):
                    pb = g % 2
                    g += 1
                    for kk in range(4):
                        k = kg * 4 + kk
                        P.op("pe", lambda e, b=b, pb=pb, kk=kk, k=k: e.transpose(
                            pt[pb][:, kk * 128:(kk + 1) * 128], xt[b][:, k * 128:(k + 1) * 128], C["ident"][:]),
                            reads=[rx[b], self.RC], writes=[rp[pb]])
                    for kk in range(4):
                        k = kg * 4 + kk
                        P.op("act", lambda e, pb=pb, kk=kk, k=k, r=r, tt=tt: e.activation(
                            out=self.hT[:, k, tt * 128:(tt + 1) * 128], in_=pt[pb][:, kk * 128:(kk + 1) * 128],
                            func=AF.Identity, scale=self.modT[:, 16 + k, r:r + 1], bias=self.modT[:, k, r:r + 1]),
                            reads=[rp[pb], self.Rmod], writes=[self.RhT])
            P.barrier()

    def phase_proj(self, l):
        P, C, I, S = self.P, self.C, self.I, self.S
        RS = self.RS
        scale = 128 ** -0.5
        with ExitStack() as ph:
            wst1 = self.sb(ph, "wst", [128, 16, 512])
            wst = [wst1, wst1]
            rws1 = Res("wst0")
            rws = [rws1, rws1]
            wbf = [self.sb(ph, "wbf", [128, 16, 512], BF16) for _ in range(2)]
            rwb = [Res("wbf0"), Res("wbf1")]
            cos = self.sb(ph, "cos", [128, T])
            sin = self.sb(ph, "sin", [128, T])
            rcs = Res("cossin")
            P.dma(cos[:], I["cos"], writes=[rcs])
            P.dma(sin[:], I["sin"], writes=[rcs])
            pa = [self.ps(ph, "pa", [128, 512]) for _ in range(2)]
            rpa = [Res("pa0"), Res("pa1")]
            pb_ = [self.ps(ph, "pb", [128, 512]) for _ in range(2)]
            rpb = [Res("pb0"), Res("pb1")]
            ptp = self.ps(ph, "ptp", [128, 512], BF16)
            rptp = Res("ptp")
            qb = self.sb(ph, "qb", [128, 512], BF16)
            rqb = Res("qb")
            t1 = self.sb(ph, "t1", [128, 512])
            t2 = self.sb(ph, "t2", [128, 512])
            rt1, rt2 = Res("t1"), Res("t2")
            qrot = self.sb(ph, "qrot", [128, T], BF16)
            rqrot = Res("qrot")
            vst = [self.sb(ph, "vst", [128, 4, 129], BF16) for _ in range(2)]
            rvst = [Res("vst0"), Res("vst1")]
            zst = [self.sb(ph, "zst", [128, 512], BF16) for _ in range(2)]
            rzst = [Res("zst0"), Res("zst1")]
            rawp = self.sb(ph, "rawp", [128, PADW])
            acc = self.sb(ph, "acc", [128, PADW])
            sact = self.sb(ph, "sact", [128, PADW], BF16)
            rraw, racc, rsact = Res("rawp"), Res("acc"), Res("sact")
            xtm = self.sb(ph, "xtm", [128, 4, 128], BF16)
            rxtm = Res("xtm")
            cwT = self.sb(ph, "cwT", [128, 60])
            cbT = self.sb(ph, "cbT", [128, 12])
            rcw = Res("cw")
            dtb = self.sb(ph, "dtb", [128, 32])
            dtt = self.sb(ph, "dtt", [128, 32])
            dt2 = self.sb(ph, "dt2", [128, 32])
            rdtb, rdtt, rdt2 = Res("dtb"), Res("dtt"), Res("dt2")
            for b in range(2):
                P.op("pool", lambda e, b=b: e.memset(vst[b][:], 1.0), writes=[rvst[b]])
            P.op("pool", lambda e: e.memset(rawp[:], 0.0), writes=[rraw])
            self.load_vecT(ph, cwT[:], I["conv_w"][l], 60, rcw)
            self.load_vecT(ph, cbT[:], I["conv_b"][l], 12, rcw)
            P.dma(dtb[:], I["dt_bias"][l:l + 1, :].partition_broadcast(128), writes=[rdtb])
            wv = I["w_in"][l].rearrange("(k p) n -> p k n", p=128)
            nblk = 12
            pcount = [0]

            def load_w(cb):
                b = cb % 2
                n = 512 if cb < 11 else 32
                P.dma(wst[b][:, :, 0:n], wv[:, :, cb * 512:cb * 512 + n], writes=[rws[b]])
                P.op("pool", lambda e: e.tensor_copy(wbf[b][:, 0:8, 0:n], wst[b][:, 0:8, 0:n]), reads=[rws[b]], writes=[rwb[b]])
                P.op("dve", lambda e: e.tensor_copy(wbf[b][:, 8:16, 0:n], wst[b][:, 8:16, 0:n]), reads=[rws[b]], writes=[rwb[b]])

            def fm_mm(b, c0, t0, n, pbuf, rpbuf):
                for k in range(16):
                    P.op("pe", lambda e, k=k: e.matmul(pbuf[:, 0:n], wbf[b][:, k, c0:c0 + 128], self.hT[:, k, t0:t0 + n],
                                                       start=(k == 0), stop=(k == 15)),
                         reads=[rwb[b], self.RhT], writes=[rpbuf])

            def tm_mm(b, tt, n, pbuf, rpbuf):
                for k in range(16):
                    P.op("pe", lambda e, k=k: e.matmul(pbuf[:, 0:n], self.hT[:, k, tt * 128:(tt + 1) * 128], wbf[b][:, k, 0:n],
                                                       start=(k == 0), stop=(k == 15)),
                         reads=[rwb[b], self.RhT], writes=[rpbuf])

            only = os.environ.get("PROJ_BLOCKS")
            only = None if only is None else set(int(v) for v in only.split(","))
            load_w(0)
            for cb in range(nblk):
                b = cb % 2
                if cb + 1 < nblk:
                    load_w(cb + 1)
                if only is not None and cb not in only:
                    continue
                if cb < 4:
                    dst = S["qT"] if cb < 2 else S["kT"]
                    rdst = RS["qT"] if cb < 2 else RS["kT"]
                    sc_ = scale if cb < 2 else 1.0
                    for hh in range(4):
                        h = (cb % 2) * 4 + hh
                        for (t0, n) in TBLK:
                            i = pcount[0] % 2
                            pcount[0] += 1
                            fm_mm(b, hh * 128, t0, n, pa[i], rpa[i])
                            QP = int(os.environ.get("QP", "9"))
                            P.op("act", lambda e: e.copy(qb[:, 0:n], pa[i][:, 0:n]), reads=[rpa[i]], writes=[rqb])
                            if QP >= 2:
                                P.op("pe", lambda e: e.matmul(pb_[i][:, 0:n], C["permb"][:], qb[:, 0:n], start=True, stop=True),
                                     reads=[rqb, self.RC], writes=[rpb[i]])
                            if QP >= 3:
                                P.op("dve", lambda e: e.scalar_tensor_tensor(out=t1[:, 0:n], in0=pa[i][:, 0:n], scalar=sc_,
                                                                             in1=cos[:, t0:t0 + n], op0=ALU.mult, op1=ALU.mult),
                                     reads=[rpa[i], rcs, rqb], writes=[rt1])
                                P.op("dve", lambda e: e.scalar_tensor_tensor(out=t2[:, 0:n], in0=pb_[i][:, 0:n], scalar=sc_,
                                                                             in1=sin[:, t0:t0 + n], op0=ALU.mult, op1=ALU.mult),
                                     reads=[rpb[i], rcs], writes=[rt2])
                            if QP >= 4:
                                P.op("pool", lambda e: e.tensor_tensor(out=qrot[:, t0:t0 + n], in0=t1[:, 0:n], in1=t2[:, 0:n], op=ALU.add),
                                     reads=[rt1, rt2], writes=[rqrot])
                        if QP < 5:
                            continue
                        P.dma(dst[h], qrot[:], reads=[rqrot], writes=[rdst])
                elif cb < 6:
                    for tt in range(NT):
                        i = pcount[0] % 2
                        pcount[0] += 1
                        tm_mm(b, tt, 512, pa[i], rpa[i])
                        P.op("act", lambda e: e.copy(vst[i][:, :, 0:128], pa[i][:].rearrange("p (h d) -> p h d", d=128)),
                             reads=[rpa[i]], writes=[rvst[i]])
                        h0 = (cb - 4) * 4
                        P.dma(S["vaug"][tt, :, h0:h0 + 4, :], vst[i][:], reads=[rvst[i]], writes=[RS["vaug"]])
                elif cb < 8:
                    for tt in range(NT):
                        i = pcount[0] % 2
                        pcount[0] += 1
                        tm_mm(b, tt, 512, pa[i], rpa[i])
                        P.op("act", lambda e: e.activation(out=zst[i][:], in_=pa[i][:], func=AF.Silu),
                             reads=[rpa[i]], writes=[rzst[i]])
                        c0 = (cb - 6) * 512
                        P.dma(S["zs"][tt * 128:(tt + 1) * 128, c0:c0 + 512], zst[i][:], reads=[rzst[i]], writes=[RS["zs"]])
                elif cb < 11:
                    for cc in range(4):
                        j = (cb - 8) * 4 + cc
                        for (t0, n) in TBLK:
                            i = pcount[0] % 2
                            pcount[0] += 1
                            fm_mm(b, cc * 128, t0, n, pa[i], rpa[i])
                            c_ = tokcol(t0)
                            P.op("act", lambda e: e.copy(rawp[:, c_:c_ + n], pa[i][:, 0:n]), reads=[rpa[i]], writes=[rraw])
                        W = PADW - 4
                        P.op("dve", lambda e: e.tensor_scalar(out=acc[:, 2:2 + W], in0=rawp[:, 0:W], scalar1=cwT[:, j:j + 1],
                                                              scalar2=None, op0=ALU.mult),
                             reads=[rraw, rcw], writes=[racc])
                        for kk in range(1, 5):
                            P.op("dve", lambda e, kk=kk: e.scalar_tensor_tensor(
                                out=acc[:, 2:2 + W], in0=rawp[:, kk:kk + W], scalar=cwT[:, kk * 12 + j:kk * 12 + j + 1],
                                in1=acc[:, 2:2 + W], op0=ALU.mult, op1=ALU.add), reads=[rraw, rcw, racc], writes=[racc])
                        P.op("act", lambda e: e.activation(out=sact[:, 2:2 + W], in_=acc[:, 2:2 + W], func=AF.Silu,
                                                           bias=cbT[:, j:j + 1]), reads=[racc, rcw], writes=[rsact])
                        if j >= 8:
                            g = (j - 8) % 2
                            dst, rdst = (S["bT"], RS["bT"]) if j < 10 else (S["cT"], RS["cT"])
                            P.dma(dst[g, :, 0:TC], sact[:, 2:2 + TC], reads=[rsact], writes=[rdst])
                            P.dma(dst[g, :, TC:T], sact[:, 262:262 + TL], reads=[rsact], writes=[rdst])
                        if j < 10:
                            for tg in range(0, NT, 4):
                                nt_ = min(4, NT - tg)
                                for q in range(nt_):
                                    c_ = tokcol((tg + q) * 128)
                                    P.op("pe", lambda e, q=q, c_=c_: e.transpose(ptp[:, q * 128:(q + 1) * 128],
                                                                                   sact[:, c_:c_ + 128], C["identb"][:]),
                                         reads=[rsact, self.RC], writes=[rptp])
                                P.op("dve", lambda e: e.tensor_copy(xtm[:, 0:nt_, :],
                                                                    ptp[:, 0:nt_ * 128].rearrange("p (q c) -> p q c", c=128)),
                                     reads=[rptp], writes=[rxtm])
                                if j < 8:
                                    dv = S["xs"][tg * 128:(tg + nt_) * 128, j * 128:(j + 1) * 128]
                                    rd = RS["xs"]
                                else:
                                    dv = S["btm"][tg * 128:(tg + nt_) * 128, (j - 8) * 128:(j - 7) * 128]
                                    rd = RS["btm"]
                                P.dma(dv.rearrange("(q p) c -> p q c", p=128), xtm[:, 0:nt_, :], reads=[rxtm], writes=[rd])
                else:
                    for tt in range(NT):
                        i = pcount[0] % 2
                        pcount[0] += 1
                        tm_mm(b, tt, 32, pa[i], rpa[i])
                        P.op("dve", lambda e: e.tensor_tensor(out=dtt[:], in0=pa[i][:, 0:32], in1=dtb[:], op=ALU.add),
                             reads=[rpa[i], rdtb], writes=[rdtt])
                        P.op("act", lambda e: e.activation(out=dtt[:], in_=dtt[:], func=AF.Exp), reads=[rdtt], writes=[rdtt])
                        P.op("act", lambda e: e.activation(out=dt2[:], in_=dtt[:], func=AF.Ln, bias=1.0), reads=[rdtt], writes=[rdt2])
                        P.dma(S["dtd"][tt * 128:(tt + 1) * 128, :], dt2[:], reads=[rdt2], writes=[RS["dtd"]])
            P.barrier()

    def phase_attn(self, l):
        P, C, I, S = self.P, self.C, self.I, self.S
        RS = self.RS
        with ExitStack() as ph:
            qT = [self.sb(ph, "qT", [128, T], BF16) for _ in range(2)]
            kT = [self.sb(ph, "kT", [128, T], BF16) for _ in range(2)]
            va = [self.sb(ph, "va", [128, NT, 129], BF16) for _ in range(2)]
            bs = [self.sb(ph, "bs", [128, 5, 5, 128]) for _ in range(2)]
            bb = [self.sb(ph, "bb", [128, 5, 5, 128], BF16) for _ in range(2)]
            rin = [Res("ain0"), Res("ain1")]
            rbs = [Res("bs0"), Res("bs1")]
            rbb = [Res("bb0"), Res("bb1")]
            psA = [self.ps(ph, "psA", [128, 512]) for _ in range(2)]
            psB = [self.ps(ph, "psB", [128, 512]) for _ in range(2)]
            rpsA = [Res("psA0"), Res("psA1")]
            rpsB = [Res("psB0"), Res("psB1")]
            po = [self.ps(ph, "po", [128, 129]) for _ in range(2)]
            rpo = [Res("po0"), Res("po1")]
            pT = [self.sb(ph, "pT", [128, 7, 128], BF16) for _ in range(2)]
            rpT = [Res("pT0"), Res("pT1")]
            rc = [self.sb(ph, "rc", [128, 1]) for _ in range(2)]
            rrc = [Res("rc0"), Res("rc1")]
            ao = [self.sb(ph, "ao", [128, 128]) for _ in range(2)]
            rao = [Res("ao0"), Res("ao1")]

            def load_head(h):
                b = h % 2
                P.dma(qT[b][:], S["qT"][h], reads=[RS["qT"]], writes=[rin[b]])
                P.dma(kT[b][:], S["kT"][h], reads=[RS["kT"]], writes=[rin[b]])
                P.dma(va[b][:], S["vaug"][:, :, h, :].rearrange("t p c -> p t c"), reads=[RS["vaug"]], writes=[rin[b]])
                P.dma(bs[b][:], I["biasT"][l, h].rearrange("v k c q -> k v c q"), writes=[rbs[b]])
                P.op("pool", lambda e: e.tensor_copy(bb[b][:], bs[b][:]), reads=[rbs[b]], writes=[rbb[b]])

            load_head(0)
            n = 0
            for h in range(8):
                b = h % 2
                if h + 1 < 8:
                    load_head(h + 1)
                qtiles = list(range(2, NT)) + ([0, 1] if self.with_ctx else [])
                for qt in qtiles:
                    i = n % 2
                    n += 1
                    if qt >= 2:
                        pi = qt - 2
                        v = pair_variant(pi)
                        c0 = pair_c0(pi)
                        chunks = [(2 + c0 + c, c) for c in range(5)] + [(0, None), (1, None)]
                    else:
                        chunks = [(0, None), (1, None)]
                    nch = len(chunks)
                    for ci, (kt, bc) in enumerate(chunks):
                        pz, rpz = (psA[i], rpsA[i]) if ci < 4 else (psB[i], rpsB[i])
                        col = (ci % 4) * 128
                        P.op("pe", lambda e, kt=kt, bc=bc, pz=pz, col=col: e.matmul(
                            pz[:, col:col + 128], kT[b][:, kt * 128:(kt + 1) * 128], qT[b][:, qt * 128:(qt + 1) * 128],
                            start=True, stop=(bc is None)), reads=[rin[b]], writes=[rpz])
                        if bc is not None:
                            P.op("pe", lambda e, bc=bc, pz=pz, col=col: e.matmul(
                                pz[:, col:col + 128], C["identb"][:], bb[b][:, v, bc, :], start=False, stop=True),
                                reads=[rbb[b], self.RC], writes=[rpz])
                    na = min(nch, 4)
                    P.op("act", lambda e: e.activation(out=pT[i][:, 0:na, :], in_=psA[i][:, 0:na * 128].rearrange("p (c q) -> p c q", q=128),
                                                       func=AF.Exp), reads=[rpsA[i]], writes=[rpT[i]])
                    if nch > 4:
                        nb_ = nch - 4
                        P.op("act", lambda e: e.activation(out=pT[i][:, 4:4 + nb_, :],
                                                           in_=psB[i][:, 0:nb_ * 128].rearrange("p (c q) -> p c q", q=128),
                                                           func=AF.Exp), reads=[rpsB[i]], writes=[rpT[i]])
                    for ci, (kt, bc) in enumerate(chunks):
                        P.op("pe", lambda e, ci=ci, kt=kt: e.matmul(po[i][:], pT[i][:, ci, :], va[b][:, kt, :],
                                                                     start=(ci == 0), stop=(ci == nch - 1)),
                             reads=[rpT[i], rin[b]], writes=[rpo[i]])
                    P.op("dve", lambda e: e.reciprocal(rc[i][:], po[i][:, 128:129]), reads=[rpo[i]], writes=[rrc[i]])
                    P.op("dve", lambda e: e.tensor_scalar(out=ao[i][:], in0=po[i][:, 0:128], scalar1=rc[i][:, 0:1], scalar2=None,
                                                          op0=ALU.mult), reads=[rpo[i], rrc[i]], writes=[rao[i]])
                    P.dma(S["att"][qt * 128:(qt + 1) * 128, h * 128:(h + 1) * 128], ao[i][:], reads=[rao[i]], writes=[RS["att"]])
            P.barrier()

    def phase_ssd(self, l):
        P, C, I, S = self.P, self.C, self.I, self.S
        RS = self.RS
        with ExitStack() as ph:
            alog = self.sb(ph, "alog", [128, 32])
            aneg = self.sb(ph, "aneg", [128, 32])
            dsk = self.sb(ph, "dsk", [128, 32])
            dsum = self.sb(ph, "dsum", [128, 16])
            rpar = Res("ssdpar")
            P.dma(alog[:], I["a_log"][l:l + 1, :].partition_broadcast(128), writes=[rpar])
            P.dma(dsk[:], I["d_skip"][l:l + 1, :].partition_broadcast(128), writes=[rpar])
            P.op("act", lambda e: e.activation(out=aneg[:], in_=alog[:], func=AF.Exp), reads=[rpar], writes=[rpar])
            P.op("dve", lambda e: e.tensor_scalar(out=aneg[:], in0=aneg[:], scalar1=-1.0, scalar2=None, op0=ALU.mult),
                 reads=[rpar], writes=[rpar])
            P.op("dve", lambda e: e.tensor_tensor(out=dsum[:], in0=dsk[:, 0:16], in1=dsk[:, 16:32], op=ALU.add),
                 reads=[rpar], writes=[rpar])
            H = self.sb(ph, "H", [128, 16, 64])
            Hb = self.sb(ph, "Hb", [128, 16, 64], BF16)
            rH, rHb = Res("H"), Res("Hb")
            NB = 2
            xs = [self.sb(ph, "xs", [128, 16, 64], BF16) for _ in range(NB)]
            btm = [self.sb(ph, "btm", [128, 256], BF16) for _ in range(NB)]
            bT = [self.sb(ph, "bT", [128, 2, 128], BF16) for _ in range(NB)]
            cT = [self.sb(ph, "cT", [128, 2, 128], BF16) for _ in range(NB)]
            dtt = [self.sb(ph, "dtt", [128, 32]) for _ in range(NB)]
            yfi = [self.sb(ph, "yfi", [128, 1024]) for _ in range(NB)]
            zsi = [self.sb(ph, "zsi", [128, 1024], BF16) for _ in range(NB)]
            rin = [Res("sin%d" % i) for i in range(NB)]
            ryfi = [Res("yfi%d" % i) for i in range(NB)]
            adt = self.sb(ph, "adt", [128, 16])
            cs = self.sb(ph, "cs", [128, 16])
            tot = self.sb(ph, "tot", [128, 16])
            ecs = self.sb(ph, "ecs", [128, 16])
            wdec = self.sb(ph, "wdec", [128, 16])
            dec = self.sb(ph, "dec", [128, 16])
            dtw = self.sb(ph, "dtw", [128, 16])
            rsm = Res("ssdsmall")
            lh = self.sb(ph, "lh", [128, 16, 128])
            rlh = Res("lh")
            eD = self.sb(ph, "eD", [128, 16, 128])
            reD = Res("eD")
            gtm = self.sb(ph, "gtm", [128, 2, 128])
            rgtm = Res("gtm")
            mT = self.sb(ph, "mT", [128, 16, 128], BF16)
            rmT = Res("mT")
            xd = self.sb(ph, "xd", [128, 16, 64], BF16)
            xdw = self.sb(ph, "xdw", [128, 16, 64], BF16)
            rxd, rxdw = Res("xd"), Res("xdw")
            yo = self.sb(ph, "yo", [128, 1024])
            ryo = Res("yo")
            yt = self.sb(ph, "yt", [128, 1024])
            ryt = Res("yt")
            pcs = self.ps(ph, "pcs", [128, 32])
            rpcs = Res("pcs")
            pD = [self.ps(ph, "pD", [128, 512]) for _ in range(4)]
            rpD = [Res("pD%d" % i) for i in range(4)]
            pG = self.ps(ph, "pG", [128, 256])
            rpG = Res("pG")
            pY = self.ps(ph, "pY", [128, 1024])
            rpY = Res("pY")

            for d in range(2):
                order = list(range(NT)) if d == 0 else [1, 0] + list(range(NT - 1, 1, -1))
                m_l = C["m_gt"] if d == 0 else C["m_lt"]
                m_r = C["m_le"] if d == 0 else C["m_ge"]
                m_m = C["m_le"] if d == 0 else C["m_ge"]
                P.op("pool", lambda e: e.memset(H[:], 0.0), reads=[rH], writes=[rH])
                P.op("pool", lambda e: e.memset(Hb[:], 0.0), reads=[rHb], writes=[rHb])

                def load_chunk(ci):
                    c = order[ci]
                    b = ci % NB
                    sl = slice(c * 128, (c + 1) * 128)
                    P.dma(xs[b][:].rearrange("p h d -> p (h d)"), S["xs"][sl, :], reads=[RS["xs"]], writes=[rin[b]])
                    P.dma(btm[b][:], S["btm"][sl, :], reads=[RS["btm"]], writes=[rin[b]])
                    P.dma(bT[b][:], S["bT"][:, :, sl].rearrange("g n t -> n g t"), reads=[RS["bT"]], writes=[rin[b]])
                    P.dma(cT[b][:], S["cT"][:, :, sl].rearrange("g n t -> n g t"), reads=[RS["cT"]], writes=[rin[b]])
                    P.dma(dtt[b][:], S["dtd"][sl, :], reads=[RS["dtd"]], writes=[rin[b]])
                    if d == 1:
                        P.dma(yfi[b][:], S["yf"][sl, :], reads=[RS["yf"]], writes=[ryfi[b]])
                        P.dma(zsi[b][:], S["zs"][sl, :], reads=[RS["zs"]], writes=[ryfi[b]])

                load_chunk(0)
                for ci in range(NT):
                    c = order[ci]
                    b = ci % NB
                    if ci + 1 < NT:
                        load_chunk(ci + 1)
                    need_y = self.with_ctx or c >= 2
                    dts = dtt[b][:, d * 16:(d + 1) * 16]
                    P.op("dve", lambda e: e.tensor_tensor(out=adt[:], in0=dts, in1=aneg[:, d * 16:(d + 1) * 16], op=ALU.mult),
                         reads=[rin[b], rpar, rsm], writes=[rsm])
                    P.op("pe", lambda e: e.matmul(pcs[:, 0:16], C["m_le"][:], adt[:], start=True, stop=True),
                         reads=[rsm, self.RC], writes=[rpcs])
                    P.op("pe", lambda e: e.matmul(pcs[:, 16:32], C["ones"][:], adt[:], start=True, stop=True),
                         reads=[rsm, self.RC], writes=[rpcs])
                    P.op("dve", lambda e: e.tensor_copy(cs[:], pcs[:, 0:16]), reads=[rpcs, rsm], writes=[rsm])
                    P.op("dve", lambda e: e.tensor_copy(tot[:], pcs[:, 16:32]), reads=[rpcs, rsm], writes=[rsm])
                    if d == 0:
                        P.op("act", lambda e: e.activation(out=ecs[:], in_=cs[:], func=AF.Exp), reads=[rsm], writes=[rsm])
                        P.op("dve", lambda e: e.tensor_tensor(out=wdec[:], in0=tot[:], in1=cs[:], op=ALU.subtract),
                             reads=[rsm], writes=[rsm])
                        P.op("act", lambda e: e.activation(out=wdec[:], in_=wdec[:], func=AF.Exp), reads=[rsm], writes=[rsm])
                    else:
                        P.op("dve", lambda e: e.tensor_tensor(out=wdec[:], in0=cs[:], in1=adt[:], op=ALU.subtract),
                             reads=[rsm], writes=[rsm])
                        P.op("dve", lambda e: e.tensor_tensor(out=ecs[:], in0=tot[:], in1=wdec[:], op=ALU.subtract),
                             reads=[rsm], writes=[rsm])
                        P.op("act", lambda e: e.activation(out=wdec[:], in_=wdec[:], func=AF.Exp), reads=[rsm], writes=[rsm])
                        P.op("act", lambda e: e.activation(out=ecs[:], in_=ecs[:], func=AF.Exp), reads=[rsm], writes=[rsm])
                    P.op("act", lambda e: e.activation(out=dec[:], in_=tot[:], func=AF.Exp), reads=[rsm], writes=[rsm])
                    P.op("dve", lambda e: e.tensor_tensor(out=dtw[:], in0=dts, in1=wdec[:], op=ALU.mult),
                         reads=[rin[b], rsm], writes=[rsm])
                    P.op("pool", lambda e: e.tensor_tensor(out=xd[:], in0=xs[b][:], in1=dts.unsqueeze(2).to_broadcast([128, 16, 64]),
                                                           op=ALU.mult), reads=[rin[b]], writes=[rxd])
                    P.op("pool", lambda e: e.tensor_tensor(out=xdw[:], in0=xs[b][:], in1=dtw[:].unsqueeze(2).to_broadcast([128, 16, 64]),
                                                           op=ALU.mult), reads=[rin[b], rsm], writes=[rxdw])
                    if need_y:
                        P.op("dve", lambda e: e.tensor_tensor(out=lh[:], in0=adt[:].unsqueeze(2).to_broadcast([128, 16, 128]),
                                                              in1=m_l[:].unsqueeze(1).to_broadcast([128, 16, 128]), op=ALU.mult),
                             reads=[rsm, self.RC], writes=[rlh])
                        for hh in range(16):
                            P.op("pe", lambda e, hh=hh: e.matmul(pD[hh // 4][:, (hh % 4) * 128:(hh % 4 + 1) * 128], lh[:, hh, :], m_r[:],
                                                                 start=True, stop=True), reads=[rlh, self.RC], writes=[rpD[hh // 4]])
                        for q in range(4):
                            P.op("act", lambda e, q=q: e.activation(out=eD[:, q * 4:(q + 1) * 4, :],
                                                                    in_=pD[q][:].rearrange("p (h l) -> p h l", l=128), func=AF.Exp),
                                 reads=[rpD[q]], writes=[reD])
                        for g in range(2):
                            P.op("pe", lambda e, g=g: e.matmul(pG[:, g * 128:(g + 1) * 128], bT[b][:, g, :], cT[b][:, g, :],
                                                               start=True, stop=True), reads=[rin[b]], writes=[rpG])
                        P.op("dve", lambda e: e.tensor_tensor(out=gtm[:], in0=pG[:].rearrange("p (g l) -> p g l", l=128),
                                                              in1=m_m[:].unsqueeze(1).to_broadcast([128, 2, 128]), op=ALU.mult),
                             reads=[rpG, self.RC], writes=[rgtm])
                        for g in range(2):
                            P.op("dve", lambda e, g=g: e.tensor_tensor(
                                out=mT[:, g * 8:(g + 1) * 8, :], in0=eD[:, g * 8:(g + 1) * 8, :],
                                in1=gtm[:, g:g + 1, :].to_broadcast([128, 8, 128]), op=ALU.mult),
                                reads=[reD, rgtm], writes=[rmT])
                        for hh in range(16):
                            P.op("pe", lambda e, hh=hh: e.matmul(pY[:, hh * 64:(hh + 1) * 64], mT[:, hh, :], xd[:, hh, :],
                                                                 start=True, stop=True), reads=[rmT, rxd], writes=[rpY])
                        for g in range(2):
                            P.op("pe", lambda e, g=g: e.matmul(pD[g][:], cT[b][:, g, :], Hb[:, g * 8:(g + 1) * 8, :].rearrange("p h d -> p (h d)"),
                                                               start=True, stop=True), reads=[rin[b], rHb], writes=[rpD[g]])
                        for g in range(2):
                            P.op("dve", lambda e, g=g: e.tensor_tensor(
                                out=yo[:, g * 512:(g + 1) * 512].rearrange("p (h d) -> p h d", d=64),
                                in0=pD[g][:].rearrange("p (h d) -> p h d", d=64),
                                in1=ecs[:, g * 8:(g + 1) * 8].unsqueeze(2).to_broadcast([128, 8, 64]), op=ALU.mult),
                                reads=[rpD[g], rsm], writes=[ryo])
                        P.op("dve", lambda e: e.tensor_tensor(out=yo[:], in0=yo[:], in1=pY[:], op=ALU.add),
                             reads=[ryo, rpY], writes=[ryo])
                        sl = slice(c * 128, (c + 1) * 128)
                        if d == 0:
                            P.dma(S["yf"][sl, :], yo[:], reads=[ryo], writes=[RS["yf"]])
                        else:
                            P.op("pool", lambda e: e.tensor_tensor(out=yt[:], in0=yo[:], in1=yfi[b][:], op=ALU.add),
                                 reads=[ryo, ryfi[b]], writes=[ryt])
                            P.op("dve", lambda e: e.tensor_tensor(out=yo[:].rearrange("p (h d) -> p h d", d=64), in0=xs[b][:],
                                                                  in1=dsum[:].unsqueeze(2).to_broadcast([128, 16, 64]), op=ALU.mult),
                                 reads=[rin[b], rpar, ryo], writes=[ryo])
                            P.op("pool", lambda e: e.tensor_tensor(out=yt[:], in0=yt[:], in1=yo[:], op=ALU.add),
                                 reads=[ryo, ryt], writes=[ryt])
                            P.op("pool", lambda e: e.tensor_tensor(out=yt[:], in0=yt[:], in1=zsi[b][:], op=ALU.mult),
                                 reads=[ryfi[b], ryt], writes=[ryt])
                            P.dma(S["ysz"][sl, :], yt[:], reads=[ryt], writes=[RS["ysz"]])
                    if ci + 1 < NT:
                        for g in range(2):
                            P.op("pe", lambda e, g=g: e.matmul(pD[2 + g][:], btm[b][:, g * 128:(g + 1) * 128],
                                                               xdw[:, g * 8:(g + 1) * 8, :].rearrange("p h d -> p (h d)"),
                                                               start=True, stop=True), reads=[rin[b], rxdw], writes=[rpD[2 + g]])
                        P.op("dve", lambda e: e.tensor_tensor(out=H[:], in0=H[:], in1=dec[:].unsqueeze(2).to_broadcast([128, 16, 64]),
                                                              op=ALU.mult), reads=[rH, rsm], writes=[rH])
                        for g in range(2):
                            P.op("dve", lambda e, g=g: e.tensor_tensor(
                                out=H[:, g * 8:(g + 1) * 8, :], in0=H[:, g * 8:(g + 1) * 8, :],
                                in1=pD[2 + g][:].rearrange("p (h d) -> p h d", d=64), op=ALU.add),
                                reads=[rH, rpD[2 + g]], writes=[rH])
                        P.op("act", lambda e: e.copy(Hb[:], H[:]), reads=[rH, rHb], writes=[rHb])
                P.barrier()

    def rstd_of(self, ph, src, n, tag):
        raise NotImplementedError

    def phase_merge(self, l):
        P, C, I, S = self.P, self.C, self.I, self.S
        RS = self.RS
        with ExitStack() as ph:
            wo = self.sb(ph, "wo", [128, 16, D], BF16)
            rwo = Res("wo")
            wst = [self.sb(ph, "wst", [128, 16, 128]) for _ in range(2)]
            rws = [Res("ws0"), Res("ws1")]
            wv = I["w_out"][l].rearrange("(k p) n -> p k n", p=128)
            for cb in range(16):
                b = cb % 2
                P.dma(wst[b][:], wv[:, :, cb * 128:(cb + 1) * 128], writes=[rws[b]])
                P.op("pool" if cb % 2 else "dve", lambda e, b=b, cb=cb: e.tensor_copy(wo[:, :, cb * 128:(cb + 1) * 128], wst[b][:]),
                     reads=[rws[b]], writes=[rwo])
            beta = self.sb(ph, "beta", [128, D])
            gbc = [self.sb(ph, "gbc", [128, D]) for _ in range(2)]
            lng = self.sb(ph, "lng", [128, D])
            lnb = self.sb(ph, "lnb", [128, D])
            rbc = Res("bcast")
            P.dma(beta[:], I["beta"][l:l + 1, :].partition_broadcast(128), writes=[rbc])
            P.dma(gbc[0][:], S["gv"][0:1, :].partition_broadcast(128), reads=[RS["gv"]], writes=[rbc])
            P.dma(gbc[1][:], S["gv"][1:2, :].partition_broadcast(128), reads=[RS["gv"]], writes=[rbc])
            P.dma(lng[:], I["ln1_g"][l:l + 1, :].partition_broadcast(128), writes=[rbc])
            P.dma(lnb[:], I["ln1_b"][l:l + 1, :].partition_broadcast(128), writes=[rbc])
            cat = [self.sb(ph, "cat", [128, D]) for _ in range(2)]
            rcat = [Res("cat0"), Res("cat1")]
            xin = [self.sb(ph, "xin", [128, D]) for _ in range(2)]
            rxin = [Res("xin0"), Res("xin1")]
            catb = self.sb(ph, "catb", [128, D], BF16)
            rcatb = Res("catb")
            catT = self.sb(ph, "catT", [128, 16, 128], BF16)
            rcatT = Res("catT")
            st_ = self.sb(ph, "st", [128, 8])
            rst = Res("st")
            y = self.sb(ph, "y", [128, D])
            ry = Res("y")
            junk, rjunk = y, ry
            h2 = self.sb(ph, "h2", [128, 16, 128], BF16)
            rh2 = Res("h2")
            ptp = self.ps(ph, "ptp", [128, 4, 128], BF16)
            rptp = Res("ptp")
            pm = [self.ps(ph, "pm", [128, 512]) for _ in range(4)]
            rpm = [Res("pm%d" % i) for i in range(4)]
            pt32 = self.ps(ph, "pt32", [128, 4, 128])
            rpt32 = Res("pt32")
            tiles = list(range(NT)) if self.with_ctx else list(range(2, NT))

            def load_t(idx):
                tt = tiles[idx]
                b = idx % 2
                sl = slice(tt * 128, (tt + 1) * 128)
                P.dma(cat[b][:, 0:1024], S["att"][sl, :], reads=[RS["att"]], writes=[rcat[b]])
                P.dma(cat[b][:, 1024:2048], S["ysz"][sl, :], reads=[RS["ysz"]], writes=[rcat[b]])
                P.dma(xin[b][:], S["xres"][sl, :], reads=[RS["xres"]], writes=[rxin[b]])

            load_t(0)
            for idx, tt in enumerate(tiles):
                b = idx % 2
                r = 1 if tt < 2 else 0
                if idx + 1 < len(tiles):
                    load_t(idx + 1)
                for hf in range(2):
                    P.op("act", lambda e, hf=hf: e.activation(out=junk[:, 0:1024], in_=cat[b][:, hf * 1024:(hf + 1) * 1024],
                                                              func=AF.Square, accum_out=st_[:, hf:hf + 1]),
                         reads=[rcat[b], rjunk, rst], writes=[rjunk, rst])
                P.op("dve", lambda e: e.tensor_scalar(out=st_[:, 2:4], in0=st_[:, 0:2], scalar1=1.0 / 1024, scalar2=EPS,
                                                      op0=ALU.mult, op1=ALU.add), reads=[rst], writes=[rst])
                P.op("act", lambda e: e.activation(out=st_[:, 2:4], in_=st_[:, 2:4], func=AF.Ln), reads=[rst], writes=[rst])
                P.op("act", lambda e: e.activation(out=st_[:, 2:4], in_=st_[:, 2:4], func=AF.Exp, scale=-0.5), reads=[rst], writes=[rst])
                for hf in range(2):
                    P.op("dve", lambda e, hf=hf: e.scalar_tensor_tensor(
                        out=catb[:, hf * 1024:(hf + 1) * 1024], in0=cat[b][:, hf * 1024:(hf + 1) * 1024],
                        scalar=st_[:, 2 + hf:3 + hf], in1=beta[:, hf * 1024:(hf + 1) * 1024], op0=ALU.mult, op1=ALU.mult),
                        reads=[rcat[b], rst, rbc, rcatb], writes=[rcatb])
                for kg in range(4):
                    for kk in range(4):
                        k = kg * 4 + kk
                        P.op("pe", lambda e, kk=kk, k=k: e.transpose(ptp[:, kk, :], catb[:, k * 128:(k + 1) * 128], C["identb"][:]),
                             reads=[rcatb, self.RC], writes=[rptp])
                    P.op("act", lambda e, kg=kg: e.copy(catT[:, kg * 4:(kg + 1) * 4, :], ptp[:]), reads=[rptp, rcatT], writes=[rcatT])
                for nb_ in range(4):
                    for k in range(16):
                        P.op("pe", lambda e, nb_=nb_, k=k: e.matmul(pm[nb_][:], catT[:, k, :], wo[:, k, nb_ * 512:(nb_ + 1) * 512],
                                                                    start=(k == 0), stop=(k == 15)),
                             reads=[rcatT, rwo], writes=[rpm[nb_]])
                for nb_ in range(4):
                    cs_ = slice(nb_ * 512, (nb_ + 1) * 512)
                    P.op("dve", lambda e, nb_=nb_, cs_=cs_: e.tensor_tensor(out=y[:, cs_], in0=pm[nb_][:], in1=gbc[r][:, cs_], op=ALU.mult),
                         reads=[rpm[nb_], rbc, ry], writes=[ry])
                P.op("dve", lambda e: e.scalar_tensor_tensor(out=y[:], in0=xin[b][:], scalar=ALPHA, in1=y[:], op0=ALU.mult, op1=ALU.add),
                     reads=[rxin[b], ry], writes=[ry])
                self.layernorm(y, ry, st_, rst, xin[b], rxin[b], lng, lnb, rbc)
                P.dma(S["xres"][tt * 128:(tt + 1) * 128, :], y[:], reads=[ry], writes=[RS["xres"]])
                for kg in range(4):
                    for kk in range(4):
                        k = kg * 4 + kk
                        P.op("pe", lambda e, kk=kk, k=k: e.transpose(pt32[:, kk, :], y[:, k * 128:(k + 1) * 128], C["ident"][:]),
                             reads=[ry, self.RC], writes=[rpt32])
                    for kk in range(4):
                        k = kg * 4 + kk
                        P.op("act", lambda e, kk=kk, k=k: e.activation(out=h2[:, k, :], in_=pt32[:, kk, :], func=AF.Identity,
                                                                       scale=self.modT[:, 64 + k, r:r + 1], bias=self.modT[:, 48 + k, r:r + 1]),
                             reads=[rpt32, self.Rmod, rh2], writes=[rh2])
                P.dma(S["h2T"][:, :, tt * 128:(tt + 1) * 128].rearrange("k p t -> p k t"), h2[:], reads=[rh2], writes=[RS["h2T"]])
            P.barrier()

    def layernorm(self, y, ry, st_, rst, junk, rjunk, lng, lnb, rbc, junk_n=None):
        P = self.P
        P.op("dve", lambda e: e.reduce_sum(out=st_[:, 4:5], in_=y[:], axis=AX.X), reads=[ry, rst], writes=[rst])
        if junk_n is None:
            P.op("act", lambda e: e.activation(out=junk[:], in_=y[:], func=AF.Square, accum_out=st_[:, 5:6]),
                 reads=[ry, rjunk, rst], writes=[rjunk, rst])
        else:
            npc = D // 512
            for pc_ in range(npc):
                P.op("act", lambda e, pc_=pc_: e.activation(out=junk[:, 0:512], in_=y[:, pc_ * 512:(pc_ + 1) * 512], func=AF.Square,
                                                            accum_out=st_[:, 7:8] if pc_ else st_[:, 5:6]),
                     reads=[ry, rjunk, rst], writes=[rjunk, rst])
                if pc_:
                    P.op("dve", lambda e: e.tensor_tensor(out=st_[:, 5:6], in0=st_[:, 5:6], in1=st_[:, 7:8], op=ALU.add),
                         reads=[rst], writes=[rst])
        P.op("dve", lambda e: e.tensor_scalar(out=st_[:, 4:6], in0=st_[:, 4:6], scalar1=1.0 / D, scalar2=None, op0=ALU.mult),
             reads=[rst], writes=[rst])
        P.op("dve", lambda e: e.tensor_tensor(out=st_[:, 6:7], in0=st_[:, 4:5], in1=st_[:, 4:5], op=ALU.mult), reads=[rst], writes=[rst])
        P.op("dve", lambda e: e.tensor_tensor(out=st_[:, 6:7], in0=st_[:, 5:6], in1=st_[:, 6:7], op=ALU.subtract), reads=[rst], writes=[rst])
        P.op("dve", lambda e: e.tensor_scalar(out=st_[:, 6:7], in0=st_[:, 6:7], scalar1=EPS, scalar2=None, op0=ALU.add),
             reads=[rst], writes=[rst])
        P.op("act", lambda e: e.activation(out=st_[:, 6:7], in_=st_[:, 6:7], func=AF.Ln), reads=[rst], writes=[rst])
        P.op("act", lambda e: e.activation(out=st_[:, 6:7], in_=st_[:, 6:7], func=AF.Exp, scale=-0.5), reads=[rst], writes=[rst])
        P.op("dve", lambda e: e.tensor_scalar(out=y[:], in0=y[:], scalar1=st_[:, 4:5], scalar2=st_[:, 6:7],
                                              op0=ALU.subtract, op1=ALU.mult), reads=[ry, rst], writes=[ry])
        P.op("pool", lambda e: e.tensor_tensor(out=y[:], in0=y[:], in1=lng[:], op=ALU.mult), reads=[ry, rbc], writes=[ry])
        P.op("pool", lambda e: e.tensor_tensor(out=y[:], in0=y[:], in1=lnb[:], op=ALU.add), reads=[ry, rbc], writes=[ry])

    def phase_route(self, l):
        P, C, I, S = self.P, self.C, self.I, self.S
        RS = self.RS
        tiles = list(range(NT)) if self.with_ctx else list(range(2, NT))
        with ExitStack() as ph:
            wq = self.sb(ph, "wq", [128, 16, D], BF16)
            rwq = Res("wq")
            wst = [self.sb(ph, "wst", [128, 16, 128]) for _ in range(2)]
            rws = [Res("ws0"), Res("ws1")]
            wv = I["peer_wq"][l].rearrange("(k p) n -> p k n", p=128)
            for cb in range(16):
                b = cb % 2
                P.dma(wst[b][:], wv[:, :, cb * 128:(cb + 1) * 128], writes=[rws[b]])
                P.op("pool" if cb % 2 else "dve", lambda e, b=b, cb=cb: e.tensor_copy(wq[:, :, cb * 128:(cb + 1) * 128], wst[b][:]),
                     reads=[rws[b]], writes=[rwq])
            keysT = self.sb(ph, "keysT", [128, 16, 128])
            rkT = Res("keysT")
            kst = self.sb(ph, "kst", [128, 16, 128])
            rkst = Res("kst")
            pr4 = [self.ps(ph, "prx", [128, 4, 128]) for _ in range(2)]
            rpr4 = [Res("prx0"), Res("prx1")]
            P.dma(kst[:], I["peer_keys"][l].rearrange("c k d -> k c d"), writes=[rkst])
            for cg in range(4):
                i = cg % 2
                for cc in range(4):
                    c = cg * 4 + cc
                    P.op("pe", lambda e, c=c, cc=cc: e.transpose(pr4[i][:, cc, :], kst[:, c, :], C["ident"][:]),
                         reads=[rkst, self.RC], writes=[rpr4[i]])
                P.op("dve", lambda e, cg=cg: e.tensor_copy(keysT[:, cg * 4:(cg + 1) * 4, :], pr4[i][:]), reads=[rpr4[i]], writes=[rkT])
            h2 = [self.sb(ph, "h2", [128, 16, 128], BF16) for _ in range(2)]
            rh2 = [Res("h2a"), Res("h2b")]
            qf = self.sb(ph, "qf", [128, 16, 128])
            rqf = Res("qf")
            sall = self.sb(ph, "sall", [128, 16, 128])
            rsall = Res("sall")
            wk = self.sb(ph, "wk", [128, 256])
            rwk = Res("wk")
            top = self.sb(ph, "top", [128, 16, 16])
            rtop = Res("top")
            cand = self.sb(ph, "cand", [128, 256])
            rcand = Res("cand")
            best = self.sb(ph, "best", [128, 8, 16])
            rbest = Res("best")
            sm = self.sb(ph, "sm", [128, 4, 8])
            rsm = Res("sm")
            j16 = self.sb(ph, "j16", [128, 16])
            rj16 = Res("j16")
            s2d = [self.sb(ph, "s2d", [128, 16, 128]) for _ in range(2)]
            rs2d = [Res("s2d0"), Res("s2d1")]
            ee = [self.sb(ph, "ee", [128, 16, 128]) for _ in range(2)]
            ree = [Res("ee0"), Res("ee1")]
            gh = [self.sb(ph, "gh", [128, 16, 128], BF16) for _ in range(8)]
            rgh = [Res("gh%d" % i) for i in range(8)]
            gto = [self.sb(ph, "gto", [128, 16, 128], BF16) for _ in range(2)]
            rgto = [Res("gto0"), Res("gto1")]
            pgacc = self.ps(ph, "prg", [128, 16, 128])
            rpg = Res("prg")

            def load_t(idx):
                tt = tiles[idx]
                P.dma(h2[idx % 2][:], S["h2T"][:, :, tt * 128:(tt + 1) * 128].rearrange("k p t -> p k t"),
                      reads=[RS["h2T"]], writes=[rh2[idx % 2]])

            load_t(0)
            n = 0
            for idx, tt in enumerate(tiles):
                hb = idx % 2
                if idx + 1 < len(tiles):
                    load_t(idx + 1)
                for cg in range(4):
                    i = cg % 2
                    for cc in range(4):
                        c = cg * 4 + cc
                        for k in range(16):
                            P.op("pe", lambda e, c=c, cc=cc, k=k: e.matmul(pr4[i][:, cc, :], wq[:, k, c * 128:(c + 1) * 128], h2[hb][:, k, :],
                                                                          start=(k == 0), stop=(k == 15)),
                                 reads=[rwq, rh2[hb]], writes=[rpr4[i]])
                    P.op("act", lambda e, cg=cg: e.copy(qf[:, cg * 4:(cg + 1) * 4, :], pr4[i][:]), reads=[rpr4[i]], writes=[rqf])
                for cg in range(4):
                    i = cg % 2
                    for cc in range(4):
                        c = cg * 4 + cc
                        P.op("pe", lambda e, c=c, cc=cc: e.matmul(pr4[i][:, cc, :], qf[:, c, :], keysT[:, c, :], start=True, stop=True),
                             reads=[rqf, rkT], writes=[rpr4[i]])
                    P.op("act", lambda e, cg=cg: e.copy(sall[:, cg * 4:(cg + 1) * 4, :], pr4[i][:]), reads=[rpr4[i]], writes=[rsall])
                for c in range(16):
                    P.op("dve", lambda e, c=c: e.max(out=top[:, c, 0:8], in_=sall[:, c, :]), reads=[rsall], writes=[rtop])
                    P.op("dve", lambda e, c=c: e.match_replace(out=wk[:, 0:128], in_to_replace=top[:, c, 0:8], in_values=sall[:, c, :],
                                                               imm_value=-1e30), reads=[rsall, rtop], writes=[rwk])
                    P.op("dve", lambda e, c=c: e.max(out=top[:, c, 8:16], in_=wk[:, 0:128]), reads=[rwk], writes=[rtop])
                for hp in range(8):
                    P.op("dve", lambda e, hp=hp: e.tensor_tensor(
                        out=cand[:].rearrange("p (i j) -> p i j", j=16), in0=top[:, 2 * hp, :].unsqueeze(2).to_broadcast([128, 16, 16]),
                        in1=top[:, 2 * hp + 1, :].unsqueeze(1).to_broadcast([128, 16, 16]), op=ALU.add), reads=[rtop], writes=[rcand])
                    P.op("dve", lambda e, hp=hp: e.max(out=best[:, hp, 0:8], in_=cand[:]), reads=[rcand], writes=[rbest])
                    P.op("dve", lambda e, hp=hp: e.match_replace(out=wk[:], in_to_replace=best[:, hp, 0:8], in_values=cand[:], imm_value=-1e30),
                         reads=[rcand, rbest], writes=[rwk])
                    P.op("dve", lambda e, hp=hp: e.max(out=best[:, hp, 8:16], in_=wk[:]), reads=[rwk], writes=[rbest])
                P.op("dve", lambda e: e.tensor_scalar(out=sm[:, 0, :], in0=best[:, :, 0], scalar1=-1.0, scalar2=None, op0=ALU.mult),
                     reads=[rbest], writes=[rsm])
                for hp in range(8):
                    P.op("act", lambda e, hp=hp: e.activation(out=j16[:], in_=best[:, hp, :], func=AF.Exp, bias=sm[:, 0, hp:hp + 1],
                                                              accum_out=sm[:, 1, hp:hp + 1]), reads=[rbest, rsm, rj16], writes=[rj16, rsm])
                P.op("act", lambda e: e.activation(out=sm[:, 2, :], in_=sm[:, 1, :], func=AF.Ln), reads=[rsm], writes=[rsm])
                P.op("dve", lambda e: e.tensor_tensor(out=sm[:, 3, :], in0=sm[:, 0, :], in1=sm[:, 2, :], op=ALU.subtract),
                     reads=[rsm], writes=[rsm])
                for kb in range(8):
                    for hp in range(8):
                        i = n % 2
                        n += 1
                        P.op("pool", lambda e, hp=hp, kb=kb: e.tensor_tensor(
                            out=s2d[i][:], in0=sall[:, 2 * hp, kb * 16:(kb + 1) * 16].unsqueeze(2).to_broadcast([128, 16, 128]),
                            in1=sall[:, 2 * hp + 1, :].unsqueeze(1).to_broadcast([128, 16, 128]), op=ALU.add),
                            reads=[rsall], writes=[rs2d[i]])
                        P.op("act", lambda e, hp=hp: e.activation(out=ee[i][:], in_=s2d[i][:], func=AF.Exp, bias=sm[:, 3, hp:hp + 1]),
                             reads=[rs2d[i], rsm], writes=[ree[i]])
                        P.op("dve", lambda e, hp=hp: e.scalar_tensor_tensor(out=gh[hp][:], in0=s2d[i][:], scalar=best[:, hp, 15:16],
                                                                           in1=ee[i][:], op0=ALU.is_ge, op1=ALU.mult),
                             reads=[rs2d[i], ree[i], rbest], writes=[rgh[hp]])
                    for kk in range(16):
                        for hp in range(8):
                            P.op("pe", lambda e, kk=kk, hp=hp: e.matmul(pgacc[:, kk, :], gh[hp][:, kk, :], C["identb"][:],
                                                                        start=(hp == 0), stop=(hp == 7)),
                                 reads=[rgh[hp], self.RC], writes=[rpg])
                    o_ = kb % 2
                    P.op("act", lambda e: e.copy(gto[o_][:], pgacc[:]), reads=[rpg], writes=[rgto[o_]])
                    P.dma(S["gT"][kb * 16:(kb + 1) * 16, :, tt * 128:(tt + 1) * 128].rearrange("a k t -> k a t"), gto[o_][:],
                          reads=[rgto[o_]], writes=[RS["gT"]])
            P.barrier()

    def phase_peer(self, l):
        P, C, I, S = self.P, self.C, self.I, self.S
        RS = self.RS
        tiles = list(range(NT)) if self.with_ctx else list(range(2, NT))
        groups = [tiles[i:i + 6] for i in range(0, len(tiles), 6)]
        SC = 4
        with ExitStack() as ph:
            acc = self.sb(ph, "oacc", [128, 6, D])
            racc = Res("oacc")
            h2g = self.sb(ph, "h2g", [128, 16, 768], BF16)
            rh2g = Res("h2g")
            ust2 = [self.sb(ph, "ust", [128, D]) for _ in range(2)]
            vst2 = [self.sb(ph, "vst", [128, D]) for _ in range(2)]
            ub = self.sb(ph, "ub", [128, D], BF16)
            rust2 = [Res("ust0"), Res("ust1")]
            rvst2 = [Res("vst0"), Res("vst1")]
            rub = Res("ub")
            uT = [self.sb(ph, "uT", [128, 16, 128], BF16) for _ in range(SC)]
            vb = [self.sb(ph, "vb", [128, D], BF16) for _ in range(SC)]
            gt = [self.sb(ph, "gt", [128, 768], BF16) for _ in range(SC)]
            wt = [self.sb(ph, "wt", [128, 768], BF16) for _ in range(SC)]
            ruT = [Res("uT%d" % i) for i in range(SC)]
            rvb = [Res("vb%d" % i) for i in range(SC)]
            rgt = [Res("gt%d" % i) for i in range(SC)]
            rwt = [Res("wt%d" % i) for i in range(SC)]
            act = self.sb(ph, "act", [128, 768])
            ract = Res("act")
            ptp = self.ps(ph, "ptp", [128, 4, 128], BF16)
            rptp = Res("ptp")
            pa = [self.ps(ph, "pa", [128, 512]) for _ in range(2)]
            rpa = [Res("pa0"), Res("pa1")]
            pm = self.ps(ph, "pm", [128, D])
            rpm = Res("pm0")
            gbc = [self.sb(ph, "gbc", [128, D]) for _ in range(2)]
            lng = self.sb(ph, "lng", [128, D])
            lnb = self.sb(ph, "lnb", [128, D])
            rbc = Res("bcast")
            P.dma(gbc[0][:], S["gv"][2:3, :].partition_broadcast(128), reads=[RS["gv"]], writes=[rbc])
            P.dma(gbc[1][:], S["gv"][3:4, :].partition_broadcast(128), reads=[RS["gv"]], writes=[rbc])
            P.dma(lng[:], I["ln2_g"][l:l + 1, :].partition_broadcast(128), writes=[rbc])
            P.dma(lnb[:], I["ln2_b"][l:l + 1, :].partition_broadcast(128), writes=[rbc])
            xin = self.sb(ph, "xin", [128, D])
            rxin = Res("xin")
            y = self.sb(ph, "y", [128, D])
            ry = Res("y")
            st_ = self.sb(ph, "st", [128, 8])
            rst = Res("st")
            na = 0
            for grp in groups:
                ng = len(grp)
                nt_ = ng * 128
                t0 = grp[0] * 128
                P.dma(h2g[:, :, 0:nt_], S["h2T"][:, :, t0:t0 + nt_].rearrange("k p t -> p k t"), reads=[RS["h2T"]], writes=[rh2g])
                P.op("pool", lambda e: e.memset(acc[:], 0.0), reads=[racc], writes=[racc])
                for sc0 in range(0, 128, SC):
                    for si in range(SC):
                        ec = sc0 + si
                        ust, vst, rust, rvst = ust2[ec % 2], vst2[ec % 2], rust2[ec % 2], rvst2[ec % 2]
                        P.dma(ust[:], I["peer_u"][l, ec * 128:(ec + 1) * 128, :], writes=[rust])
                        P.dma(vst[:], I["peer_v"][l, ec * 128:(ec + 1) * 128, :], writes=[rvst])
                        P.dma(gt[si][:, 0:nt_], S["gT"][ec, :, t0:t0 + nt_], reads=[RS["gT"]], writes=[rgt[si]])
                        P.op("dve", lambda e: e.tensor_copy(ub[:], ust[:]), reads=[rust], writes=[rub])
                        P.op("pool", lambda e, si=si: e.tensor_copy(vb[si][:], vst[:]), reads=[rvst], writes=[rvb[si]])
                        for kg in range(4):
                            for kk in range(4):
                                k = kg * 4 + kk
                                P.op("pe", lambda e, kk=kk, k=k: e.transpose(ptp[:, kk, :], ub[:, k * 128:(k + 1) * 128], C["identb"][:]),
                                     reads=[rub, self.RC], writes=[rptp])
                            P.op("act", lambda e, kg=kg, si=si: e.copy(uT[si][:, kg * 4:(kg + 1) * 4, :], ptp[:]),
                                 reads=[rptp], writes=[ruT[si]])
                        for tb0 in range(0, nt_, 512):
                            nb_ = min(512, nt_ - tb0)
                            i = na % 2
                            na += 1
                            for k in range(16):
                                P.op("pe", lambda e, k=k, si=si: e.matmul(pa[i][:, 0:nb_], uT[si][:, k, :], h2g[:, k, tb0:tb0 + nb_],
                                                                          start=(k == 0), stop=(k == 15)),
                                     reads=[ruT[si], rh2g], writes=[rpa[i]])
                            P.op("act", lambda e: e.activation(out=act[:, tb0:tb0 + nb_], in_=pa[i][:, 0:nb_], func=AF.Gelu),
                                 reads=[rpa[i]], writes=[ract])
                        P.op("dve", lambda e, si=si: e.tensor_tensor(out=wt[si][:, 0:nt_], in0=act[:, 0:nt_], in1=gt[si][:, 0:nt_], op=ALU.mult),
                             reads=[ract, rgt[si]], writes=[rwt[si]])
                    for j in range(ng):
                        for nb4 in range(4):
                            for si in range(SC):
                                P.op("pe", lambda e, j=j, nb4=nb4, si=si: e.matmul(
                                    pm[:, nb4 * 512:(nb4 + 1) * 512], wt[si][:, j * 128:(j + 1) * 128], vb[si][:, nb4 * 512:(nb4 + 1) * 512],
                                    start=(si == 0), stop=(si == SC - 1)), reads=[rwt[si], rvb[si]], writes=[rpm])
                        P.op("dve", lambda e, j=j: e.tensor_tensor(out=acc[:, j, :], in0=acc[:, j, :], in1=pm[:], op=ALU.add),
                             reads=[racc, rpm], writes=[racc])
                for j, tt in enumerate(grp):
                    r = 1 if tt < 2 else 0
                    P.dma(xin[:], S["xres"][tt * 128:(tt + 1) * 128, :], reads=[RS["xres"]], writes=[rxin])
                    P.op("dve", lambda e, j=j, r=r: e.tensor_tensor(out=y[:], in0=acc[:, j, :], in1=gbc[r][:], op=ALU.mult),
                         reads=[racc, rbc, ry], writes=[ry])
                    P.op("dve", lambda e: e.scalar_tensor_tensor(out=y[:], in0=xin[:], scalar=ALPHA, in1=y[:], op0=ALU.mult, op1=ALU.add),
                         reads=[rxin, ry], writes=[ry])
                    self.layernorm(y, ry, st_, rst, act, ract, lng, lnb, rbc, junk_n=768)
                    P.dma(S["xres"][tt * 128:(tt + 1) * 128, :], y[:], reads=[ry], writes=[RS["xres"]])
            P.barrier()


def make_inputs(inputs, depth=DEPTH):
    f = lambda a: np.ascontiguousarray(np.asarray(a, dtype=np.float32))
    cst = host_consts()
    shared = {
        "ada_w": f(inputs["ada_w"][:depth]),
        "ada_b": f(inputs["ada_b"][:depth]).reshape(depth, 96, 128),
        "w_in": f(inputs["w_in"][:depth]),
        "conv_w": f(inputs["conv_w"][:depth]).reshape(depth, 5, 12, 128).reshape(depth, 60, 128),
        "conv_b": f(inputs["conv_b"][:depth]).reshape(depth, 12, 128),
        "a_log": f(inputs["a_log"][:depth]).reshape(depth, 32),
        "dt_bias": f(inputs["dt_bias"][:depth]).reshape(depth, 32),
        "d_skip": f(inputs["d_skip"][:depth]).reshape(depth, 32),
        "biasT": host_bias(f(inputs["rpb"][:depth])),
        "beta": np.concatenate([f(inputs["beta_attn"][:depth]), f(inputs["beta_ssm"][:depth])], axis=1),
        "w_out": f(inputs["w_out"][:depth]),
        "ln1_g": f(inputs["ln1_g"][:depth]), "ln1_b": f(inputs["ln1_b"][:depth]),
        "ln2_g": f(inputs["ln2_g"][:depth]), "ln2_b": f(inputs["ln2_b"][:depth]),
        "peer_wq": f(inputs["peer_wq"][:depth]),
        "peer_keys": f(inputs["peer_keys"][:depth]).reshape(depth, 16, 128, 128),
        "peer_u": f(inputs["peer_u"][:depth]),
        "peer_v": f(inputs["peer_v"][:depth]),
    }
    shared.update(cst)
    return shared


def kernel(**inputs):
    shared = make_inputs(inputs)
    B = inputs["x"].shape[0]
    nc = Builder().build()
    in_maps = []
    for b in range(B):
        m = dict(shared)
        m["x"] = np.ascontiguousarray(inputs["x"][b], dtype=np.float32)
        m["ctx"] = np.ascontiguousarray(inputs["ctx"][b], dtype=np.float32)
        m["c2"] = np.concatenate([np.asarray(inputs["c"][b], np.float32).reshape(16, 128),
                                  np.asarray(inputs["c_ctx"], np.float32).reshape(16, 128)], axis=0)
        in_maps.append(m)
    res = run_bass_kernel_spmd(nc, in_maps, core_ids=list(range(B)))
    return np.stack([np.asarray(r["out"], dtype=np.float32) for r in res.results], axis=0)
```

```python
import os
import re
import numpy as np
from contextlib import ExitStack
import concourse.bass as bass
import concourse.mybir as mybir
from concourse.bass_utils import run_bass_kernel_spmd

F32 = mybir.dt.float32
BF16 = mybir.dt.bfloat16
AF = mybir.ActivationFunctionType
ALU = mybir.AluOpType
AX = mybir.AxisListType

D = 2048
TC = 256
TL = 2048
T = TC + TL
NT = T // 128
PROJ = 5664
DEPTH = 4
ALPHA = (2 * DEPTH) ** 0.25
EPS = 1e-6
NEG = -30000.0
PADW = 2312
TBLK = [(0, 256), (256, 512), (768, 512), (1280, 512), (1792, 512)]


def tokcol(t):
    return 2 + t if t < TC else 262 + (t - TC)


class Res:
    __slots__ = ("name", "writer", "readers")

    def __init__(self, name):
        self.name = name
        self.writer = None
        self.readers = []


class Prog:
    NDMA = 24

    def __init__(self, nc, stack):
        self.nc = nc
        self.eng = {"pe": nc.tensor, "dve": nc.vector, "act": nc.scalar,
                    "pool": nc.gpsimd, "sp": nc.sync}
        self.sems = {}
        self.count = {}
        for k in list(self.eng) + ["d%d" % i for i in range(self.NDMA)]:
            self.sems[k] = stack.enter_context(nc.semaphore("s_" + k))
            self.count[k] = 0
        self.seen = {e: {} for e in self.eng}
        self.dma_rr = 0
        self.ninst = 0
        self.uid = 0

    def _wait(self, e, k, v):
        if self.seen[e].get(k, 0) >= v:
            return
        self.eng[e].wait_ge(self.sems[k], v)
        self.seen[e][k] = v
        self.ninst += 1

    _PS = re.compile(r"^(pc|lvp|pmod|pg|ptr\d|pa\d|pb\d|ptp|psA\d|psB\d|po\d|pcs|pD\d|pG|pY|pm\d|pt32|pr\w*)$")

    def _deps(self, reads, writes):
        deps = {}
        for r in reads:
            if r.writer is not None:
                k, v = r.writer
                if deps.get(k, 0) < v:
                    deps[k] = v
            if self._PS.match(r.name):
                for k, v in r.readers:
                    if deps.get(k, 0) < v:
                        deps[k] = v
        for w in writes:
            if w.writer is not None:
                k, v = w.writer
                if deps.get(k, 0) < v:
                    deps[k] = v
            for k, v in w.readers:
                if deps.get(k, 0) < v:
                    deps[k] = v
        return deps

    @staticmethod
    def _record(key, val, reads, writes):
        for r in reads:
            r.readers.append((key, val))
            if len(r.readers) > 48:
                m = {}
                for k, v in r.readers:
                    if m.get(k, 0) < v:
                        m[k] = v
                r.readers = list(m.items())
        for w in writes:
            w.writer = (key, val)
            w.readers = []

    def op(self, e, fn, reads=(), writes=()):
        deps = self._deps(reads, writes)
        for k, v in deps.items():
            if e == "pe" and k == "pe":
                continue
            self._wait(e, k, v)
        ins = fn(self.eng[e])
        self.count[e] += 1
        ins.then_inc(self.sems[e], 1)
        self.ninst += 1
        self._record(e, self.count[e], reads, writes)
        return ins

    def dma(self, out, in_, reads=(), writes=(), e="sp", **kw):
        k = "d%d" % self.dma_rr
        self.dma_rr = (self.dma_rr + 1) % self.NDMA
        deps = self._deps(reads, writes)
        if self.count[k] > 0 and deps.get(k, 0) < self.count[k]:
            deps[k] = self.count[k]
        for kk, v in deps.items():
            self._wait(e, kk, v)
        ins = self.eng[e].dma_start(out=out, in_=in_, **kw)
        self.count[k] += 16
        ins.then_inc(self.sems[k], 16)
        self.ninst += 1
        self._record(k, self.count[k], reads, writes)
        return ins

    def barrier(self, engines=("pe", "dve", "act", "pool", "sp")):
        for e in engines:
            for k in self.count:
                if self.count[k] > 0:
                    self._wait(e, k, self.count[k])


def host_consts():
    c = {}
    c["ident"] = np.eye(128, dtype=np.float32)
    perm = np.zeros((128, 128), np.float32)
    for i in range(128):
        a, r = divmod(i, 64)
        p = a * 64 + (r + 32) % 64
        perm[p, i] = 1.0
    c["perm"] = perm
    t = np.arange(TL)
    pos = np.stack([t // 64, t % 64], -1).astype(np.float32)
    inv = (10000.0 ** (-np.arange(0, 64, 2, dtype=np.float32) / 64)).astype(np.float32)
    ang = pos[:, :, None] * inv
    cos = np.ones((128, T), np.float32)
    sin = np.zeros((128, T), np.float32)
    for a in range(2):
        cos[a * 64:a * 64 + 32, TC:] = np.cos(ang[:, a, :]).T
        cos[a * 64 + 32:a * 64 + 64, TC:] = np.cos(ang[:, a, :]).T
        sin[a * 64:a * 64 + 32, TC:] = -np.sin(ang[:, a, :]).T
        sin[a * 64 + 32:a * 64 + 64, TC:] = np.sin(ang[:, a, :]).T
    c["cos"] = cos
    c["sin"] = sin
    k = np.arange(128)[:, None]
    l = np.arange(128)[None, :]
    c["m_le"] = (k <= l).astype(np.float32)
    c["m_ge"] = (k >= l).astype(np.float32)
    c["m_gt"] = (k > l).astype(np.float32)
    c["m_lt"] = (k < l).astype(np.float32)
    c["ones"] = np.ones((128, 128), np.float32)
    return c


VAR_I = [0, 1, 7, 14, 15]


def pair_variant(i):
    return {0: 0, 1: 1, 14: 3, 15: 4}.get(i, 2)


def pair_c0(i):
    return min(max(i - 2, 0), 11)


def host_bias(rpb):
    L = rpb.shape[0]
    out = np.empty((L, 8, 5, 128, 5, 128), np.float32)
    kl = np.arange(128)[:, None, None]
    ch = np.arange(5)[None, :, None]
    ql = np.arange(128)[None, None, :]
    for v, i in enumerate(VAR_I):
        c0 = pair_c0(i)
        qr = 2 * i + ql // 64
        qc = ql % 64
        kr = 2 * (c0 + ch) + kl // 64
        kc = kl % 64
        rs = np.clip(qr - 4, 0, 24)
        cs = np.clip(qc - 8, 0, 48)
        valid = (kr >= rs) & (kr < rs + 8) & (kc >= cs) & (kc < cs + 16)
        dr = np.clip(kr - qr + 7, 0, 14)
        dc = np.clip(kc - qc + 15, 0, 30)
        dr, dc, valid = np.broadcast_arrays(dr, dc, valid)
        g = rpb[:, :, dr, dc]
        out[:, :, v] = np.where(valid[None, None], g, np.float32(NEG))
    return out


class Builder:
    def __init__(self, depth=DEPTH, dbg=(), stop=None):
        self.depth = depth
        self.dbg = set(dbg)
        self.stop = stop
        self.nc = bass.Bass("TRN2", target_bir_lowering=False)
        self.uid = 0

    def din(self, name, shape, dt=F32):
        return self.nc.dram_tensor(name, list(shape), dt, kind="ExternalInput").ap()

    def dscr(self, name, shape, dt=F32):
        kind = "ExternalOutput" if name in self.dbg else "Internal"
        return self.nc.dram_tensor(name, list(shape), dt, kind=kind).ap()

    def sb(self, st, name, shape, dt=F32):
        self.uid += 1
        return st.enter_context(self.nc.sbuf_tensor("%s_%d" % (name, self.uid), list(shape), dt))

    def ps(self, st, name, shape, dt=F32):
        self.uid += 1
        return st.enter_context(self.nc.psum_tensor("%s_%d" % (name, self.uid), list(shape), dt))

    def build(self):
        nc = self.nc
        dp = self.depth
        I = {}
        I["x"] = self.din("x", [TL, D])
        I["ctx"] = self.din("ctx", [TC, D])
        I["c2"] = self.din("c2", [32, 128])
        I["ada_w"] = self.din("ada_w", [dp, D, 6 * D])
        I["ada_b"] = self.din("ada_b", [dp, 96, 128])
        I["w_in"] = self.din("w_in", [dp, D, PROJ])
        I["conv_w"] = self.din("conv_w", [dp, 60, 128])
        I["conv_b"] = self.din("conv_b", [dp, 12, 128])
        I["a_log"] = self.din("a_log", [dp, 32])
        I["dt_bias"] = self.din("dt_bias", [dp, 32])
        I["d_skip"] = self.din("d_skip", [dp, 32])
        I["biasT"] = self.din("biasT", [dp, 8, 5, 128, 5, 128])
        I["beta"] = self.din("beta", [dp, D])
        I["w_out"] = self.din("w_out", [dp, D, D])
        for n in ("ln1_g", "ln1_b", "ln2_g", "ln2_b"):
            I[n] = self.din(n, [dp, D])
        I["peer_wq"] = self.din("peer_wq", [dp, D, D])
        I["peer_keys"] = self.din("peer_keys", [dp, 16, 128, 128])
        ne = 128 if (self.stop is not None and self.stop != "peer") else 16384
        I["peer_u"] = self.din("peer_u", [dp, ne, D])
        I["peer_v"] = self.din("peer_v", [dp, ne, D])
        for n in ("ident", "perm", "m_le", "m_ge", "m_gt", "m_lt", "ones"):
            I[n] = self.din(n, [128, 128])
        I["cos"] = self.din("cos", [128, T])
        I["sin"] = self.din("sin", [128, T])
        self.I = I
        self.out = nc.dram_tensor("out", [TL, D], F32, kind="ExternalOutput").ap()

        S = {}
        S["xres"] = self.dscr("xres", [T, D])
        S["gv"] = self.dscr("gv", [4, D])
        S["qT"] = self.dscr("qT", [8, 128, T], BF16)
        S["kT"] = self.dscr("kT", [8, 128, T], BF16)
        S["vaug"] = self.dscr("vaug", [NT, 128, 8, 129], BF16)
        S["zs"] = self.dscr("zs", [T, 1024], BF16)
        S["xs"] = self.dscr("xs", [T, 1024], BF16)
        S["btm"] = self.dscr("btm", [T, 256], BF16)
        S["bT"] = self.dscr("bT", [2, 128, T], BF16)
        S["cT"] = self.dscr("cT", [2, 128, T], BF16)
        S["att"] = self.dscr("att", [T, 1024])
        S["yf"] = self.dscr("yf", [T, 1024])
        S["ysz"] = self.dscr("ysz", [T, 1024])
        S["h2T"] = self.dscr("h2T", [16, 128, T], BF16)
        S["gT"] = self.dscr("gT", [128, 128, T], BF16)
        S["dtd"] = self.dscr("dtd", [T, 32])
        S["fD"] = self.dscr("fD", [T, D])
        self.S = S
        self.RS = {k: Res(k) for k in S}

        with ExitStack() as st:
            self.P = P = Prog(nc, st)
            self.st = st
            C = {}
            self.RC = Res("consts")
            for n in ("ident", "perm", "m_le", "m_ge", "m_gt", "m_lt", "ones"):
                C[n] = self.sb(st, n, [128, 128])
                P.dma(C[n][:], I[n], writes=[self.RC])
            C["identb"] = self.sb(st, "identb", [128, 128], BF16)
            C["permb"] = self.sb(st, "permb", [128, 128], BF16)
            C["m_leb"] = self.sb(st, "m_leb", [128, 128], BF16)
            C["m_geb"] = self.sb(st, "m_geb", [128, 128], BF16)
            P.op("dve", lambda e: e.tensor_copy(C["identb"][:], C["ident"][:]), reads=[self.RC], writes=[self.RC])
            P.op("dve", lambda e: e.tensor_copy(C["permb"][:], C["perm"][:]), reads=[self.RC], writes=[self.RC])
            P.op("dve", lambda e: e.tensor_copy(C["m_leb"][:], C["m_le"][:]), reads=[self.RC], writes=[self.RC])
            P.op("dve", lambda e: e.tensor_copy(C["m_geb"][:], C["m_ge"][:]), reads=[self.RC], writes=[self.RC])
            self.C = C
            P.dma(S["xres"][0:TC, :], I["ctx"], writes=[self.RS["xres"]])
            for i in range(4):
                P.dma(S["xres"][TC + i * 512:TC + (i + 1) * 512, :], I["x"][i * 512:(i + 1) * 512, :],
                      writes=[self.RS["xres"]])
            self.scT = self.sb(st, "scT", [128, 16, 2])
            self.Rsc = Res("scT")
            with ExitStack() as ph:
                c2 = self.sb(ph, "c2", [32, 128])
                pc = self.ps(ph, "pc", [128, 32])
                r1, r2 = Res("c2"), Res("pc")
                P.dma(c2[:], I["c2"], writes=[r1])
                P.op("pe", lambda e: e.transpose(pc[:], c2[:], C["ident"][0:32, 0:32]), reads=[r1, self.RC], writes=[r2])
                for r in range(2):
                    P.op("act", lambda e, r=r: e.activation(out=self.scT[:, :, r], in_=pc[:, r * 16:(r + 1) * 16], func=AF.Silu),
                         reads=[r2], writes=[self.Rsc])
                P.barrier()
            P.barrier()
            for l in range(dp):
                self.layer(l)
                if self.stop is not None and l == 0:
                    break
            P.barrier()
            if self.stop is None:
                for i in range(4):
                    P.dma(self.out[i * 512:(i + 1) * 512, :], S["xres"][TC + i * 512:TC + (i + 1) * 512, :],
                          reads=[self.RS["xres"]])
            P.barrier(engines=("sp",))
        return nc

    def load_vecT(self, ph, dst, src_rows, n, rdst):
        P, C = self.P, self.C
        tmp = self.sb(ph, "lv", [n, 128])
        pt = self.ps(ph, "lvp", [128, n])
        r1, r2 = Res("lv"), Res("lvp")
        P.dma(tmp[:], src_rows, writes=[r1])
        P.op("pe", lambda e: e.transpose(pt[:], tmp[:], C["ident"][0:n, 0:n]), reads=[r1, self.RC], writes=[r2])
        P.op("dve", lambda e: e.tensor_copy(dst, pt[:]), reads=[r2], writes=[rdst])

    def layer(self, l):
        P = self.P
        with_ctx = l < DEPTH - 1 if self.depth == DEPTH else (l < self.depth - 1 or self.depth == 1)
        self.with_ctx = with_ctx
        with ExitStack() as ly:
            self.modT = self.sb(ly, "modT", [128, 96, 2])
            self.Rmod = Res("modT")
            self.phase_mod(l)
            P.barrier()
            if self.stop == "mod":
                return
            with ExitStack() as l2:
                self.hT = self.sb(l2, "hT", [128, 16, T], BF16)
                self.RhT = Res("hT")
                self.phase_hT()
                P.barrier()
                if self.stop == "hT":
                    return
                self.phase_proj(l)
                P.barrier()
            if self.stop == "proj":
                return
            self.phase_attn(l)
            P.barrier()
            if self.stop == "attn":
                return
            self.phase_ssd(l)
            P.barrier()
            if self.stop == "ssd":
                return
            self.phase_merge(l)
            P.barrier()
            if self.stop == "merge":
                return
        with ExitStack() as ly:
            self.phase_route(l)
            P.barrier()
            if self.stop == "route":
                return
            self.phase_peer(l)
            P.barrier()

    def phase_mod(self, l):
        P, C, I, S = self.P, self.C, self.I, self.S
        with ExitStack() as ph:
            wt = [self.sb(ph, "adaw", [128, 16, 512]) for _ in range(2)]
            rw = [Res("adaw0"), Res("adaw1")]
            pm = self.ps(ph, "pmod", [128, 192])
            rpm = Res("pmod")
            abT = self.sb(ph, "abT", [128, 96])
            rab = Res("abT")
            self.load_vecT(ph, abT[:], I["ada_b"][l], 96, rab)
            wv = I["ada_w"][l].rearrange("(k p) n -> p k n", p=128)
            for cg in range(24):
                b = cg % 2
                P.dma(wt[b][:], wv[:, :, cg * 512:(cg + 1) * 512], writes=[rw[b]])
                for jj in range(4):
                    j = cg * 4 + jj
                    for k in range(16):
                        P.op("pe", lambda e, b=b, jj=jj, j=j, k=k: e.matmul(
                            pm[:, 2 * j:2 * j + 2], wt[b][:, k, jj * 128:(jj + 1) * 128], self.scT[:, k, :],
                            start=(k == 0), stop=(k == 15)), reads=[rw[b], self.Rsc], writes=[rpm])
            P.op("dve", lambda e: e.tensor_tensor(out=self.modT[:], in0=pm[:].rearrange("p (j r) -> p j r", r=2),
                                                  in1=abT[:].unsqueeze(2).to_broadcast([128, 96, 2]), op=ALU.add),
                 reads=[rpm, rab], writes=[self.Rmod])
            for j0 in (16, 64):
                P.op("dve", lambda e, j0=j0: e.tensor_scalar_add(self.modT[:, j0:j0 + 16, :], self.modT[:, j0:j0 + 16, :], 1.0),
                     reads=[self.Rmod], writes=[self.Rmod])
            pg = self.ps(ph, "pg", [16, 4, 128])
            gsb = self.sb(ph, "gsb", [16, 4, 128])
            rpg, rgs = Res("pg"), Res("gsb")
            for gi, (j0, r) in enumerate([(32, 0), (32, 1), (80, 0), (80, 1)]):
                P.op("pe", lambda e, gi=gi, j0=j0, r=r: e.transpose(pg[:, gi, :], self.modT[:, j0:j0 + 16, r], C["ident"][:]),
                     reads=[self.Rmod, self.RC], writes=[rpg])
            P.op("dve", lambda e: e.tensor_copy(gsb[:], pg[:]), reads=[rpg], writes=[rgs])
            P.dma(S["gv"].rearrange("g (k p) -> k g p", p=128), gsb[:], reads=[rgs], writes=[self.RS["gv"]])
            P.barrier()

    def phase_hT(self):
        P, C, S = self.P, self.C, self.S
        with ExitStack() as ph:
            xt = [self.sb(ph, "xt", [128, D]) for _ in range(2)]
            rx = [Res("xt0"), Res("xt1")]
            pt = [self.ps(ph, "ptr", [128, 512]) for _ in range(2)]
            rp = [Res("ptr0"), Res("ptr1")]
            g = 0
            for tt in range(NT):
                b = tt % 2
                r = 1 if tt < 2 else 0
                P.dma(xt[b][:], S["xres"][tt * 128:(tt + 1) * 128, :], reads=[self.RS["xres"]], writes=[rx[b]])
                for kg in range(4):
                    pb = g % 2
                    g += 1
                    for kk in range(4):
                        k = kg * 4 + kk
                        P.op("pe", lambda e, b=b, pb=pb, kk=kk, k=k: e.transpose(
                            pt[pb][:, kk * 128:(kk + 1) * 128], xt[b][:, k * 128:(k + 1) * 128], C["ident"][:]),
                            reads=[rx[b], self.RC], writes=[rp[pb]])
                    for kk in range(4):
                        k = kg * 4 + kk
                        P.op("act", lambda e, pb=pb, kk=kk, k=k, r=r, tt=tt: e.activation(
                            out=self.hT[:, k, tt * 128:(tt + 1) * 128], in_=pt[pb][:, kk * 128:(kk + 1) * 128],
                            func=AF.Identity, scale=self.modT[:, 16 + k, r:r + 1], bias=self.modT[:, k, r:r + 1]),
                            reads=[rp[pb], self.Rmod], writes=[self.RhT])
            P.barrier()

    def phase_proj(self, l):
        P, C, I, S = self.P, self.C, self.I, self.S
        RS = self.RS
        scale = 128 ** -0.5
        with ExitStack() as ph:
            wst1 = self.sb(ph, "wst", [128, 16, 512])
            wst = [wst1, wst1]
            rws1 = Res("wst0")
            rws = [rws1, rws1]
            wbf = [self.sb(ph, "wbf", [128, 16, 512], BF16) for _ in range(2)]
            rwb = [Res("wbf0"), Res("wbf1")]
            cos = self.sb(ph, "cos", [128, T])
            sin = self.sb(ph, "sin", [128, T])
            rcs = Res("cossin")
            P.dma(cos[:], I["cos"], writes=[rcs])
            P.dma(sin[:], I["sin"], writes=[rcs])
            pa = [self.ps(ph, "pa", [128, 512]) for _ in range(2)]
            rpa = [Res("pa0"), Res("pa1")]
            pb_ = [self.ps(ph, "pb", [128, 512]) for _ in range(2)]
            rpb = [Res("pb0"), Res("pb1")]
            ptp = self.ps(ph, "ptp", [128, 512], BF16)
            rptp = Res("ptp")
            qb = self.sb(ph, "qb", [128, 512], BF16)
            rqb = Res("qb")
            t1 = self.sb(ph, "t1", [128, 512])
            t2 = self.sb(ph, "t2", [128, 512])
            rt1, rt2 = Res("t1"), Res("t2")
            qrot = self.sb(ph, "qrot", [128, T], BF16)
            rqrot = Res("qrot")
            vst = [self.sb(ph, "vst", [128, 4, 129], BF16) for _ in range(2)]
            rvst = [Res("vst0"), Res("vst1")]
            zst = [self.sb(ph, "zst", [128, 512], BF16) for _ in range(2)]
            rzst = [Res("zst0"), Res("zst1")]
            rawp = self.sb(ph, "rawp", [128, PADW])
            acc = self.sb(ph, "acc", [128, PADW])
            sact = self.sb(ph, "sact", [128, PADW], BF16)
            rraw, racc, rsact = Res("rawp"), Res("acc"), Res("sact")
            xtm = self.sb(ph, "xtm", [128, 4, 128], BF16)
            rxtm = Res("xtm")
            cwT = self.sb(ph, "cwT", [128, 60])
            cbT = self.sb(ph, "cbT", [128, 12])
            rcw = Res("cw")
            dtb = self.sb(ph, "dtb", [128, 32])
            dtt = self.sb(ph, "dtt", [128, 32])
            dt2 = self.sb(ph, "dt2", [128, 32])
            rdtb, rdtt, rdt2 = Res("dtb"), Res("dtt"), Res("dt2")
            for b in range(2):
                P.op("pool", lambda e, b=b: e.memset(vst[b][:], 1.0), writes=[rvst[b]])
            P.op("pool", lambda e: e.memset(rawp[:], 0.0), writes=[rraw])
            self.load_vecT(ph, cwT[:], I["conv_w"][l], 60, rcw)
            self.load_vecT(ph, cbT[:], I["conv_b"][l], 12, rcw)
            P.dma(dtb[:], I["dt_bias"][l:l + 1, :].partition_broadcast(128), writes=[rdtb])
            wv = I["w_in"][l].rearrange("(k p) n -> p k n", p=128)
            nblk = 12
            pcount = [0]

            def load_w(cb):
                b = cb % 2
                n = 512 if cb < 11 else 32
                P.dma(wst[b][:, :, 0:n], wv[:, :, cb * 512:cb * 512 + n], writes=[rws[b]])
                P.op("pool", lambda e: e.tensor_copy(wbf[b][:, 0:8, 0:n], wst[b][:, 0:8, 0:n]), reads=[rws[b]], writes=[rwb[b]])
                P.op("dve", lambda e: e.tensor_copy(wbf[b][:, 8:16, 0:n], wst[b][:, 8:16, 0:n]), reads=[rws[b]], writes=[rwb[b]])

            def fm_mm(b, c0, t0, n, pbuf, rpbuf):
                for k in range(16):
                    P.op("pe", lambda e, k=k: e.matmul(pbuf[:, 0:n], wbf[b][:, k, c0:c0 + 128], self.hT[:, k, t0:t0 + n],
                                                       start=(k == 0), stop=(k == 15)),
                         reads=[rwb[b], self.RhT], writes=[rpbuf])

            def tm_mm(b, tt, n, pbuf, rpbuf):
                for k in range(16):
                    P.op("pe", lambda e, k=k: e.matmul(pbuf[:, 0:n], self.hT[:, k, tt * 128:(tt + 1) * 128], wbf[b][:, k, 0:n],
                                                       start=(k == 0), stop=(k == 15)),
                         reads=[rwb[b], self.RhT], writes=[rpbuf])

            only = None
            only = None if only is None else set(int(v) for v in only.split(","))
            load_w(0)
            for cb in range(nblk):
                b = cb % 2
                if cb + 1 < nblk:
                    load_w(cb + 1)
                if only is not None and cb not in only:
                    continue
                if cb < 4:
                    dst = S["qT"] if cb < 2 else S["kT"]
                    rdst = RS["qT"] if cb < 2 else RS["kT"]
                    sc_ = scale if cb < 2 else 1.0
                    for hh in range(4):
                        h = (cb % 2) * 4 + hh
                        for (t0, n) in TBLK:
                            i = pcount[0] % 2
                            pcount[0] += 1
                            fm_mm(b, hh * 128, t0, n, pa[i], rpa[i])
                            QP = 9
                            P.op("act", lambda e: e.copy(qb[:, 0:n], pa[i][:, 0:n]), reads=[rpa[i]], writes=[rqb])
                            if QP >= 2:
                                P.op("pe", lambda e: e.matmul(pb_[i][:, 0:n], C["permb"][:], qb[:, 0:n], start=True, stop=True),
                                     reads=[rqb, self.RC], writes=[rpb[i]])
                            if QP >= 3:
                                P.op("dve", lambda e: e.scalar_tensor_tensor(out=t1[:, 0:n], in0=pa[i][:, 0:n], scalar=sc_,
                                                                             in1=cos[:, t0:t0 + n], op0=ALU.mult, op1=ALU.mult),
                                     reads=[rpa[i], rcs, rqb], writes=[rt1])
                                P.op("dve", lambda e: e.scalar_tensor_tensor(out=t2[:, 0:n], in0=pb_[i][:, 0:n], scalar=sc_,
                                                                             in1=sin[:, t0:t0 + n], op0=ALU.mult, op1=ALU.mult),
                                     reads=[rpb[i], rcs], writes=[rt2])
                            if QP >= 4:
                                P.op("pool", lambda e: e.tensor_tensor(out=qrot[:, t0:t0 + n], in0=t1[:, 0:n], in1=t2[:, 0:n], op=ALU.add),
                                     reads=[rt1, rt2], writes=[rqrot])
                        if QP < 5:
                            continue
                        P.dma(dst[h], qrot[:], reads=[rqrot], writes=[rdst])
                elif cb < 6:
                    for tt in range(NT):
                        i = pcount[0] % 2
                        pcount[0] += 1
                        tm_mm(b, tt, 512, pa[i], rpa[i])
                        P.op("act", lambda e: e.copy(vst[i][:, :, 0:128], pa[i][:].rearrange("p (h d) -> p h d", d=128)),
                             reads=[rpa[i]], writes=[rvst[i]])
                        h0 = (cb - 4) * 4
                        P.dma(S["vaug"][tt, :, h0:h0 + 4, :], vst[i][:], reads=[rvst[i]], writes=[RS["vaug"]])
                elif cb < 8:
                    for tt in range(NT):
                        i = pcount[0] % 2
                        pcount[0] += 1
                        tm_mm(b, tt, 512, pa[i], rpa[i])
                        P.op("act", lambda e: e.activation(out=zst[i][:], in_=pa[i][:], func=AF.Silu),
                             reads=[rpa[i]], writes=[rzst[i]])
                        c0 = (cb - 6) * 512
                        P.dma(S["zs"][tt * 128:(tt + 1) * 128, c0:c0 + 512], zst[i][:], reads=[rzst[i]], writes=[RS["zs"]])
                elif cb < 11:
                    for cc in range(4):
                        j = (cb - 8) * 4 + cc
                        for (t0, n) in TBLK:
                            i = pcount[0] % 2
                            pcount[0] += 1
                            fm_mm(b, cc * 128, t0, n, pa[i], rpa[i])
                            c_ = tokcol(t0)
                            P.op("act", lambda e: e.copy(rawp[:, c_:c_ + n], pa[i][:, 0:n]), reads=[rpa[i]], writes=[rraw])
                        W = PADW - 4
                        P.op("dve", lambda e: e.tensor_scalar(out=acc[:, 2:2 + W], in0=rawp[:, 0:W], scalar1=cwT[:, j:j + 1],
                                                              scalar2=None, op0=ALU.mult),
                             reads=[rraw, rcw], writes=[racc])
                        for kk in range(1, 5):
                            P.op("dve", lambda e, kk=kk: e.scalar_tensor_tensor(
                                out=acc[:, 2:2 + W], in0=rawp[:, kk:kk + W], scalar=cwT[:, kk * 12 + j:kk * 12 + j + 1],
                                in1=acc[:, 2:2 + W], op0=ALU.mult, op1=ALU.add), reads=[rraw, rcw, racc], writes=[racc])
                        P.op("act", lambda e: e.activation(out=sact[:, 2:2 + W], in_=acc[:, 2:2 + W], func=AF.Silu,
                                                           bias=cbT[:, j:j + 1]), reads=[racc, rcw], writes=[rsact])
                        if j >= 8:
                            g = (j - 8) % 2
                            dst, rdst = (S["bT"], RS["bT"]) if j < 10 else (S["cT"], RS["cT"])
                            P.dma(dst[g, :, 0:TC], sact[:, 2:2 + TC], reads=[rsact], writes=[rdst])
                            P.dma(dst[g, :, TC:T], sact[:, 262:262 + TL], reads=[rsact], writes=[rdst])
                        if j < 10:
                            for tg in range(0, NT, 4):
                                nt_ = min(4, NT - tg)
                                for q in range(nt_):
                                    c_ = tokcol((tg + q) * 128)
                                    P.op("pe", lambda e, q=q, c_=c_: e.transpose(ptp[:, q * 128:(q + 1) * 128],
                                                                                   sact[:, c_:c_ + 128], C["identb"][:]),
                                         reads=[rsact, self.RC], writes=[rptp])
                                P.op("dve", lambda e: e.tensor_copy(xtm[:, 0:nt_, :],
                                                                    ptp[:, 0:nt_ * 128].rearrange("p (q c) -> p q c", c=128)),
                                     reads=[rptp], writes=[rxtm])
                                if j < 8:
                                    dv = S["xs"][tg * 128:(tg + nt_) * 128, j * 128:(j + 1) * 128]
                                    rd = RS["xs"]
                                else:
                                    dv = S["btm"][tg * 128:(tg + nt_) * 128, (j - 8) * 128:(j - 7) * 128]
                                    rd = RS["btm"]
                                P.dma(dv.rearrange("(q p) c -> p q c", p=128), xtm[:, 0:nt_, :], reads=[rxtm], writes=[rd])
                else:
                    for tt in range(NT):
                        i = pcount[0] % 2
                        pcount[0] += 1
                        tm_mm(b, tt, 32, pa[i], rpa[i])
                        P.op("dve", lambda e: e.tensor_tensor(out=dtt[:], in0=pa[i][:, 0:32], in1=dtb[:], op=ALU.add),
                             reads=[rpa[i], rdtb], writes=[rdtt])
                        P.op("act", lambda e: e.activation(out=dtt[:], in_=dtt[:], func=AF.Exp), reads=[rdtt], writes=[rdtt])
                        P.op("act", lambda e: e.activation(out=dt2[:], in_=dtt[:], func=AF.Ln, bias=1.0), reads=[rdtt], writes=[rdt2])
                        P.dma(S["dtd"][tt * 128:(tt + 1) * 128, :], dt2[:], reads=[rdt2], writes=[RS["dtd"]])
            P.barrier()

    def phase_attn(self, l):
        P, C, I, S = self.P, self.C, self.I, self.S
        RS = self.RS
        with ExitStack() as ph:
            qT = [self.sb(ph, "qT", [128, T], BF16) for _ in range(2)]
            kT = [self.sb(ph, "kT", [128, T], BF16) for _ in range(2)]
            va = [self.sb(ph, "va", [128, NT, 129], BF16) for _ in range(2)]
            bs = [self.sb(ph, "bs", [128, 5, 5, 128]) for _ in range(2)]
            bb = [self.sb(ph, "bb", [128, 5, 5, 128], BF16) for _ in range(2)]
            rin = [Res("ain0"), Res("ain1")]
            rbs = [Res("bs0"), Res("bs1")]
            rbb = [Res("bb0"), Res("bb1")]
            psA = [self.ps(ph, "psA", [128, 512]) for _ in range(2)]
            psB = [self.ps(ph, "psB", [128, 512]) for _ in range(2)]
            rpsA = [Res("psA0"), Res("psA1")]
            rpsB = [Res("psB0"), Res("psB1")]
            po = [self.ps(ph, "po", [128, 129]) for _ in range(2)]
            rpo = [Res("po0"), Res("po1")]
            pT = [self.sb(ph, "pT", [128, 7, 128], BF16) for _ in range(2)]
            rpT = [Res("pT0"), Res("pT1")]
            rc = [self.sb(ph, "rc", [128, 1]) for _ in range(2)]
            rrc = [Res("rc0"), Res("rc1")]
            ao = [self.sb(ph, "ao", [128, 128]) for _ in range(2)]
            rao = [Res("ao0"), Res("ao1")]

            def load_head(h):
                b = h % 2
                P.dma(qT[b][:], S["qT"][h], reads=[RS["qT"]], writes=[rin[b]])
                P.dma(kT[b][:], S["kT"][h], reads=[RS["kT"]], writes=[rin[b]])
                P.dma(va[b][:], S["vaug"][:, :, h, :].rearrange("t p c -> p t c"), reads=[RS["vaug"]], writes=[rin[b]])
                P.dma(bs[b][:], I["biasT"][l, h].rearrange("v k c q -> k v c q"), writes=[rbs[b]])
                P.op("pool", lambda e: e.tensor_copy(bb[b][:], bs[b][:]), reads=[rbs[b]], writes=[rbb[b]])

            load_head(0)
            n = 0
            for h in range(8):
                b = h % 2
                if h + 1 < 8:
                    load_head(h + 1)
                qtiles = list(range(2, NT)) + ([0, 1] if self.with_ctx else [])
                for qt in qtiles:
                    i = n % 2
                    n += 1
                    if qt >= 2:
                        pi = qt - 2
                        v = pair_variant(pi)
                        c0 = pair_c0(pi)
                        chunks = [(2 + c0 + c, c) for c in range(5)] + [(0, None), (1, None)]
                    else:
                        chunks = [(0, None), (1, None)]
                    nch = len(chunks)
                    for ci, (kt, bc) in enumerate(chunks):
                        pz, rpz = (psA[i], rpsA[i]) if ci < 4 else (psB[i], rpsB[i])
                        col = (ci % 4) * 128
                        P.op("pe", lambda e, kt=kt, bc=bc, pz=pz, col=col: e.matmul(
                            pz[:, col:col + 128], kT[b][:, kt * 128:(kt + 1) * 128], qT[b][:, qt * 128:(qt + 1) * 128],
                            start=True, stop=(bc is None)), reads=[rin[b]], writes=[rpz])
                        if bc is not None:
                            P.op("pe", lambda e, bc=bc, pz=pz, col=col: e.matmul(
                                pz[:, col:col + 128], C["identb"][:], bb[b][:, v, bc, :], start=False, stop=True),
                                reads=[rbb[b], self.RC], writes=[rpz])
                    na = min(nch, 4)
                    P.op("act", lambda e: e.activation(out=pT[i][:, 0:na, :], in_=psA[i][:, 0:na * 128].rearrange("p (c q) -> p c q", q=128),
                                                       func=AF.Exp), reads=[rpsA[i]], writes=[rpT[i]])
                    if nch > 4:
                        nb_ = nch - 4
                        P.op("act", lambda e: e.activation(out=pT[i][:, 4:4 + nb_, :],
                                                           in_=psB[i][:, 0:nb_ * 128].rearrange("p (c q) -> p c q", q=128),
                                                           func=AF.Exp), reads=[rpsB[i]], writes=[rpT[i]])
                    for ci, (kt, bc) in enumerate(chunks):
                        P.op("pe", lambda e, ci=ci, kt=kt: e.matmul(po[i][:], pT[i][:, ci, :], va[b][:, kt, :],
                                                                     start=(ci == 0), stop=(ci == nch - 1)),
                             reads=[rpT[i], rin[b]], writes=[rpo[i]])
                    P.op("dve", lambda e: e.reciprocal(rc[i][:], po[i][:, 128:129]), reads=[rpo[i]], writes=[rrc[i]])
                    P.op("dve", lambda e: e.tensor_scalar(out=ao[i][:], in0=po[i][:, 0:128], scalar1=rc[i][:, 0:1], scalar2=None,
                                                          op0=ALU.mult), reads=[rpo[i], rrc[i]], writes=[rao[i]])
                    P.dma(S["att"][qt * 128:(qt + 1) * 128, h * 128:(h + 1) * 128], ao[i][:], reads=[rao[i]], writes=[RS["att"]])
            P.barrier()

    def phase_ssd(self, l):
        P, C, I, S = self.P, self.C, self.I, self.S
        RS = self.RS
        with ExitStack() as ph:
            alog = self.sb(ph, "alog", [128, 32])
            aneg = self.sb(ph, "aneg", [128, 32])
            dsk = self.sb(ph, "dsk", [128, 32])
            dsum = self.sb(ph, "dsum", [128, 16])
            rpar = Res("ssdpar")
            P.dma(alog[:], I["a_log"][l:l + 1, :].partition_broadcast(128), writes=[rpar])
            P.dma(dsk[:], I["d_skip"][l:l + 1, :].partition_broadcast(128), writes=[rpar])
            P.op("act", lambda e: e.activation(out=aneg[:], in_=alog[:], func=AF.Exp), reads=[rpar], writes=[rpar])
            P.op("dve", lambda e: e.tensor_scalar(out=aneg[:], in0=aneg[:], scalar1=-1.0, scalar2=None, op0=ALU.mult),
                 reads=[rpar], writes=[rpar])
            P.op("dve", lambda e: e.tensor_tensor(out=dsum[:], in0=dsk[:, 0:16], in1=dsk[:, 16:32], op=ALU.add),
                 reads=[rpar], writes=[rpar])
            H = self.sb(ph, "H", [128, 16, 64])
            Hb = self.sb(ph, "Hb", [128, 16, 64], BF16)
            rH, rHb = Res("H"), Res("Hb")
            NB = 2
            xs = [self.sb(ph, "xs", [128, 16, 64], BF16) for _ in range(NB)]
            btm = [self.sb(ph, "btm", [128, 256], BF16) for _ in range(NB)]
            bT = [self.sb(ph, "bT", [128, 2, 128], BF16) for _ in range(NB)]
            cT = [self.sb(ph, "cT", [128, 2, 128], BF16) for _ in range(NB)]
            dtt = [self.sb(ph, "dtt", [128, 32]) for _ in range(NB)]
            yfi = [self.sb(ph, "yfi", [128, 1024]) for _ in range(NB)]
            zsi = [self.sb(ph, "zsi", [128, 1024], BF16) for _ in range(NB)]
            rin = [Res("sin%d" % i) for i in range(NB)]
            ryfi = [Res("yfi%d" % i) for i in range(NB)]
            adt = self.sb(ph, "adt", [128, 16])
            cs = self.sb(ph, "cs", [128, 16])
            tot = self.sb(ph, "tot", [128, 16])
            ecs = self.sb(ph, "ecs", [128, 16])
            wdec = self.sb(ph, "wdec", [128, 16])
            dec = self.sb(ph, "dec", [128, 16])
            dtw = self.sb(ph, "dtw", [128, 16])
            rsm = Res("ssdsmall")
            lh = self.sb(ph, "lh", [128, 16, 128])
            rlh = Res("lh")
            eD = self.sb(ph, "eD", [128, 16, 128])
            reD = Res("eD")
            gtm = self.sb(ph, "gtm", [128, 2, 128])
            rgtm = Res("gtm")
            mT = self.sb(ph, "mT", [128, 16, 128], BF16)
            rmT = Res("mT")
            xd = self.sb(ph, "xd", [128, 16, 64], BF16)
            xdw = self.sb(ph, "xdw", [128, 16, 64], BF16)
            rxd, rxdw = Res("xd"), Res("xdw")
            yo = self.sb(ph, "yo", [128, 1024])
            ryo = Res("yo")
            yt = self.sb(ph, "yt", [128, 1024])
            ryt = Res("yt")
            pcs = self.ps(ph, "pcs", [128, 32])
            rpcs = Res("pcs")
            pD = [self.ps(ph, "pD", [128, 512]) for _ in range(4)]
            rpD = [Res("pD%d" % i) for i in range(4)]
            pG = self.ps(ph, "pG", [128, 256])
            rpG = Res("pG")
            pY = self.ps(ph, "pY", [128, 1024])
            rpY = Res("pY")

            for d in range(2):
                order = list(range(NT)) if d == 0 else [1, 0] + list(range(NT - 1, 1, -1))
                m_l = C["m_gt"] if d == 0 else C["m_lt"]
                m_r = C["m_le"] if d == 0 else C["m_ge"]
                m_m = C["m_le"] if d == 0 else C["m_ge"]
                P.op("pool", lambda e: e.memset(H[:], 0.0), reads=[rH], writes=[rH])
                P.op("pool", lambda e: e.memset(Hb[:], 0.0), reads=[rHb], writes=[rHb])

                def load_chunk(ci):
                    c = order[ci]
                    b = ci % NB
                    sl = slice(c * 128, (c + 1) * 128)
                    P.dma(xs[b][:].rearrange("p h d -> p (h d)"), S["xs"][sl, :], reads=[RS["xs"]], writes=[rin[b]])
                    P.dma(btm[b][:], S["btm"][sl, :], reads=[RS["btm"]], writes=[rin[b]])
                    P.dma(bT[b][:], S["bT"][:, :, sl].rearrange("g n t -> n g t"), reads=[RS["bT"]], writes=[rin[b]])
                    P.dma(cT[b][:], S["cT"][:, :, sl].rearrange("g n t -> n g t"), reads=[RS["cT"]], writes=[rin[b]])
                    P.dma(dtt[b][:], S["dtd"][sl, :], reads=[RS["dtd"]], writes=[rin[b]])
                    if d == 1:
                        P.dma(yfi[b][:], S["yf"][sl, :], reads=[RS["yf"]], writes=[ryfi[b]])
                        P.dma(zsi[b][:], S["zs"][sl, :], reads=[RS["zs"]], writes=[ryfi[b]])

                load_chunk(0)
                for ci in range(NT):
                    c = order[ci]
                    b = ci % NB
                    if ci + 1 < NT:
                        load_chunk(ci + 1)
                    need_y = self.with_ctx or c >= 2
                    dts = dtt[b][:, d * 16:(d + 1) * 16]
                    P.op("dve", lambda e: e.tensor_tensor(out=adt[:], in0=dts, in1=aneg[:, d * 16:(d + 1) * 16], op=ALU.mult),
                         reads=[rin[b], rpar, rsm], writes=[rsm])
                    P.op("pe", lambda e: e.matmul(pcs[:, 0:16], C["m_le"][:], adt[:], start=True, stop=True),
                         reads=[rsm, self.RC], writes=[rpcs])
                    P.op("pe", lambda e: e.matmul(pcs[:, 16:32], C["ones"][:], adt[:], start=True, stop=True),
                         reads=[rsm, self.RC], writes=[rpcs])
                    P.op("dve", lambda e: e.tensor_copy(cs[:], pcs[:, 0:16]), reads=[rpcs, rsm], writes=[rsm])
                    P.op("dve", lambda e: e.tensor_copy(tot[:], pcs[:, 16:32]), reads=[rpcs, rsm], writes=[rsm])
                    if d == 0:
                        P.op("act", lambda e: e.activation(out=ecs[:], in_=cs[:], func=AF.Exp), reads=[rsm], writes=[rsm])
                        P.op("dve", lambda e: e.tensor_tensor(out=wdec[:], in0=tot[:], in1=cs[:], op=ALU.subtract),
                             reads=[rsm], writes=[rsm])
                        P.op("act", lambda e: e.activation(out=wdec[:], in_=wdec[:], func=AF.Exp), reads=[rsm], writes=[rsm])
                    else:
                        P.op("dve", lambda e: e.tensor_tensor(out=wdec[:], in0=cs[:], in1=adt[:], op=ALU.subtract),
                             reads=[rsm], writes=[rsm])
                        P.op("dve", lambda e: e.tensor_tensor(out=ecs[:], in0=tot[:], in1=wdec[:], op=ALU.subtract),
                             reads=[rsm], writes=[rsm])
                        P.op("act", lambda e: e.activation(out=wdec[:], in_=wdec[:], func=AF.Exp), reads=[rsm], writes=[rsm])
                        P.op("act", lambda e: e.activation(out=ecs[:], in_=ecs[:], func=AF.Exp), reads=[rsm], writes=[rsm])
                    P.op("act", lambda e: e.activation(out=dec[:], in_=tot[:], func=AF.Exp), reads=[rsm], writes=[rsm])
                    P.op("dve", lambda e: e.tensor_tensor(out=dtw[:], in0=dts, in1=wdec[:], op=ALU.mult),
                         reads=[rin[b], rsm], writes=[rsm])
                    P.op("pool", lambda e: e.tensor_tensor(out=xd[:], in0=xs[b][:], in1=dts.unsqueeze(2).to_broadcast([128, 16, 64]),
                                                           op=ALU.mult), reads=[rin[b]], writes=[rxd])
                    P.op("pool", lambda e: e.tensor_tensor(out=xdw[:], in0=xs[b][:], in1=dtw[:].unsqueeze(2).to_broadcast([128, 16, 64]),
                                                           op=ALU.mult), reads=[rin[b], rsm], writes=[rxdw])
                    if need_y:
                        P.op("dve", lambda e: e.tensor_tensor(out=lh[:], in0=adt[:].unsqueeze(2).to_broadcast([128, 16, 128]),
                                                              in1=m_l[:].unsqueeze(1).to_broadcast([128, 16, 128]), op=ALU.mult),
                             reads=[rsm, self.RC], writes=[rlh])
                        for hh in range(16):
                            P.op("pe", lambda e, hh=hh: e.matmul(pD[hh // 4][:, (hh % 4) * 128:(hh % 4 + 1) * 128], lh[:, hh, :], m_r[:],
                                                                 start=True, stop=True), reads=[rlh, self.RC], writes=[rpD[hh // 4]])
                        for q in range(4):
                            P.op("act", lambda e, q=q: e.activation(out=eD[:, q * 4:(q + 1) * 4, :],
                                                                    in_=pD[q][:].rearrange("p (h l) -> p h l", l=128), func=AF.Exp),
                                 reads=[rpD[q]], writes=[reD])
                        for g in range(2):
                            P.op("pe", lambda e, g=g: e.matmul(pG[:, g * 128:(g + 1) * 128], bT[b][:, g, :], cT[b][:, g, :],
                                                               start=True, stop=True), reads=[rin[b]], writes=[rpG])
                        P.op("dve", lambda e: e.tensor_tensor(out=gtm[:], in0=pG[:].rearrange("p (g l) -> p g l", l=128),
                                                              in1=m_m[:].unsqueeze(1).to_broadcast([128, 2, 128]), op=ALU.mult),
                             reads=[rpG, self.RC], writes=[rgtm])
                        for g in range(2):
                            P.op("dve", lambda e, g=g: e.tensor_tensor(
                                out=mT[:, g * 8:(g + 1) * 8, :], in0=eD[:, g * 8:(g + 1) * 8, :],
                                in1=gtm[:, g:g + 1, :].to_broadcast([128, 8, 128]), op=ALU.mult),
                                reads=[reD, rgtm], writes=[rmT])
                        for hh in range(16):
                            P.op("pe", lambda e, hh=hh: e.matmul(pY[:, hh * 64:(hh + 1) * 64], mT[:, hh, :], xd[:, hh, :],
                                                                 start=True, stop=True), reads=[rmT, rxd], writes=[rpY])
                        for g in range(2):
                            P.op("pe", lambda e, g=g: e.matmul(pD[g][:], cT[b][:, g, :], Hb[:, g * 8:(g + 1) * 8, :].rearrange("p h d -> p (h d)"),
                                                               start=True, stop=True), reads=[rin[b], rHb], writes=[rpD[g]])
                        for g in range(2):
                            P.op("dve", lambda e, g=g: e.tensor_tensor(
                                out=yo[:, g * 512:(g + 1) * 512].rearrange("p (h d) -> p h d", d=64),
                                in0=pD[g][:].rearrange("p (h d) -> p h d", d=64),
                                in1=ecs[:, g * 8:(g + 1) * 8].unsqueeze(2).to_broadcast([128, 8, 64]), op=ALU.mult),
                                reads=[rpD[g], rsm], writes=[ryo])
                        P.op("dve", lambda e: e.tensor_tensor(out=yo[:], in0=yo[:], in1=pY[:], op=ALU.add),
                             reads=[ryo, rpY], writes=[ryo])
                        sl = slice(c * 128, (c + 1) * 128)
                        if d == 0:
                            P.dma(S["yf"][sl, :], yo[:], reads=[ryo], writes=[RS["yf"]])
                        else:
                            P.op("pool", lambda e: e.tensor_tensor(out=yt[:], in0=yo[:], in1=yfi[b][:], op=ALU.add),
                                 reads=[ryo, ryfi[b]], writes=[ryt])
                            P.op("dve", lambda e: e.tensor_tensor(out=yo[:].rearrange("p (h d) -> p h d", d=64), in0=xs[b][:],
                                                                  in1=dsum[:].unsqueeze(2).to_broadcast([128, 16, 64]), op=ALU.mult),
                                 reads=[rin[b], rpar, ryo], writes=[ryo])
                            P.op("pool", lambda e: e.tensor_tensor(out=yt[:], in0=yt[:], in1=yo[:], op=ALU.add),
                                 reads=[ryo, ryt], writes=[ryt])
                            P.op("pool", lambda e: e.tensor_tensor(out=yt[:], in0=yt[:], in1=zsi[b][:], op=ALU.mult),
                                 reads=[ryfi[b], ryt], writes=[ryt])
                            P.dma(S["ysz"][sl, :], yt[:], reads=[ryt], writes=[RS["ysz"]])
                    if ci + 1 < NT:
                        for g in range(2):
                            P.op("pe", lambda e, g=g: e.matmul(pD[2 + g][:], btm[b][:, g * 128:(g + 1) * 128],
                                                               xdw[:, g * 8:(g + 1) * 8, :].rearrange("p h d -> p (h d)"),
                                                               start=True, stop=True), reads=[rin[b], rxdw], writes=[rpD[2 + g]])
                        P.op("dve", lambda e: e.tensor_tensor(out=H[:], in0=H[:], in1=dec[:].unsqueeze(2).to_broadcast([128, 16, 64]),
                                                              op=ALU.mult), reads=[rH, rsm], writes=[rH])
                        for g in range(2):
                            P.op("dve", lambda e, g=g: e.tensor_tensor(
                                out=H[:, g * 8:(g + 1) * 8, :], in0=H[:, g * 8:(g + 1) * 8, :],
                                in1=pD[2 + g][:].rearrange("p (h d) -> p h d", d=64), op=ALU.add),
                                reads=[rH, rpD[2 + g]], writes=[rH])
                        P.op("act", lambda e: e.copy(Hb[:], H[:]), reads=[rH, rHb], writes=[rHb])
                P.barrier()

    def rstd_of(self, ph, src, n, tag):
        raise NotImplementedError

    def phase_merge(self, l):
        P, C, I, S = self.P, self.C, self.I, self.S
        RS = self.RS
        with ExitStack() as ph:
            wo = self.sb(ph, "wo", [128, 16, D], BF16)
            rwo = Res("wo")
            wst = [self.sb(ph, "wst", [128, 16, 128]) for _ in range(2)]
            rws = [Res("ws0"), Res("ws1")]
            wv = I["w_out"][l].rearrange("(k p) n -> p k n", p=128)
            for cb in range(16):
                b = cb % 2
                P.dma(wst[b][:], wv[:, :, cb * 128:(cb + 1) * 128], writes=[rws[b]])
                P.op("pool" if cb % 2 else "dve", lambda e, b=b, cb=cb: e.tensor_copy(wo[:, :, cb * 128:(cb + 1) * 128], wst[b][:]),
                     reads=[rws[b]], writes=[rwo])
            beta = self.sb(ph, "beta", [128, D])
            gbc = [self.sb(ph, "gbc", [128, D]) for _ in range(2)]
            lng = self.sb(ph, "lng", [128, D])
            lnb = self.sb(ph, "lnb", [128, D])
            rbc = Res("bcast")
            P.dma(beta[:], I["beta"][l:l + 1, :].partition_broadcast(128), writes=[rbc])
            P.dma(gbc[0][:], S["gv"][0:1, :].partition_broadcast(128), reads=[RS["gv"]], writes=[rbc])
            P.dma(gbc[1][:], S["gv"][1:2, :].partition_broadcast(128), reads=[RS["gv"]], writes=[rbc])
            P.dma(lng[:], I["ln1_g"][l:l + 1, :].partition_broadcast(128), writes=[rbc])
            P.dma(lnb[:], I["ln1_b"][l:l + 1, :].partition_broadcast(128), writes=[rbc])
            cat = [self.sb(ph, "cat", [128, D]) for _ in range(2)]
            rcat = [Res("cat0"), Res("cat1")]
            xin = [self.sb(ph, "xin", [128, D]) for _ in range(2)]
            rxin = [Res("xin0"), Res("xin1")]
            catb = self.sb(ph, "catb", [128, D], BF16)
            rcatb = Res("catb")
            catT = self.sb(ph, "catT", [128, 16, 128], BF16)
            rcatT = Res("catT")
            st_ = self.sb(ph, "st", [128, 8])
            rst = Res("st")
            y = self.sb(ph, "y", [128, D])
            ry = Res("y")
            junk, rjunk = y, ry
            h2 = self.sb(ph, "h2", [128, 16, 128], BF16)
            rh2 = Res("h2")
            ptp = self.ps(ph, "ptp", [128, 4, 128], BF16)
            rptp = Res("ptp")
            pm = [self.ps(ph, "pm", [128, 512]) for _ in range(4)]
            rpm = [Res("pm%d" % i) for i in range(4)]
            pt32 = self.ps(ph, "pt32", [128, 4, 128])
            rpt32 = Res("pt32")
            tiles = list(range(NT)) if self.with_ctx else list(range(2, NT))

            def load_t(idx):
                tt = tiles[idx]
                b = idx % 2
                sl = slice(tt * 128, (tt + 1) * 128)
                P.dma(cat[b][:, 0:1024], S["att"][sl, :], reads=[RS["att"]], writes=[rcat[b]])
                P.dma(cat[b][:, 1024:2048], S["ysz"][sl, :], reads=[RS["ysz"]], writes=[rcat[b]])
                P.dma(xin[b][:], S["xres"][sl, :], reads=[RS["xres"]], writes=[rxin[b]])

            load_t(0)
            for idx, tt in enumerate(tiles):
                b = idx % 2
                r = 1 if tt < 2 else 0
                if idx + 1 < len(tiles):
                    load_t(idx + 1)
                for hf in range(2):
                    P.op("act", lambda e, hf=hf: e.activation(out=junk[:, 0:1024], in_=cat[b][:, hf * 1024:(hf + 1) * 1024],
                                                              func=AF.Square, accum_out=st_[:, hf:hf + 1]),
                         reads=[rcat[b], rjunk, rst], writes=[rjunk, rst])
                P.op("dve", lambda e: e.tensor_scalar(out=st_[:, 2:4], in0=st_[:, 0:2], scalar1=1.0 / 1024, scalar2=EPS,
                                                      op0=ALU.mult, op1=ALU.add), reads=[rst], writes=[rst])
                P.op("act", lambda e: e.activation(out=st_[:, 2:4], in_=st_[:, 2:4], func=AF.Ln), reads=[rst], writes=[rst])
                P.op("act", lambda e: e.activation(out=st_[:, 2:4], in_=st_[:, 2:4], func=AF.Exp, scale=-0.5), reads=[rst], writes=[rst])
                for hf in range(2):
                    P.op("dve", lambda e, hf=hf: e.scalar_tensor_tensor(
                        out=catb[:, hf * 1024:(hf + 1) * 1024], in0=cat[b][:, hf * 1024:(hf + 1) * 1024],
                        scalar=st_[:, 2 + hf:3 + hf], in1=beta[:, hf * 1024:(hf + 1) * 1024], op0=ALU.mult, op1=ALU.mult),
                        reads=[rcat[b], rst, rbc, rcatb], writes=[rcatb])
                for kg in range(4):
                    for kk in range(4):
                        k = kg * 4 + kk
                        P.op("pe", lambda e, kk=kk, k=k: e.transpose(ptp[:, kk, :], catb[:, k * 128:(k + 1) * 128], C["identb"][:]),
                             reads=[rcatb, self.RC], writes=[rptp])
                    P.op("act", lambda e, kg=kg: e.copy(catT[:, kg * 4:(kg + 1) * 4, :], ptp[:]), reads=[rptp, rcatT], writes=[rcatT])
                for nb_ in range(4):
                    for k in range(16):
                        P.op("pe", lambda e, nb_=nb_, k=k: e.matmul(pm[nb_][:], catT[:, k, :], wo[:, k, nb_ * 512:(nb_ + 1) * 512],
                                                                    start=(k == 0), stop=(k == 15)),
                             reads=[rcatT, rwo], writes=[rpm[nb_]])
                for nb_ in range(4):
                    cs_ = slice(nb_ * 512, (nb_ + 1) * 512)
                    P.op("dve", lambda e, nb_=nb_, cs_=cs_: e.tensor_tensor(out=y[:, cs_], in0=pm[nb_][:], in1=gbc[r][:, cs_], op=ALU.mult),
                         reads=[rpm[nb_], rbc, ry], writes=[ry])
                P.op("dve", lambda e: e.scalar_tensor_tensor(out=y[:], in0=xin[b][:], scalar=ALPHA, in1=y[:], op0=ALU.mult, op1=ALU.add),
                     reads=[rxin[b], ry], writes=[ry])
                self.layernorm(y, ry, st_, rst, xin[b], rxin[b], lng, lnb, rbc)
                P.dma(S["xres"][tt * 128:(tt + 1) * 128, :], y[:], reads=[ry], writes=[RS["xres"]])
                for kg in range(4):
                    for kk in range(4):
                        k = kg * 4 + kk
                        P.op("pe", lambda e, kk=kk, k=k: e.transpose(pt32[:, kk, :], y[:, k * 128:(k + 1) * 128], C["ident"][:]),
                             reads=[ry, self.RC], writes=[rpt32])
                    for kk in range(4):
                        k = kg * 4 + kk
                        P.op("act", lambda e, kk=kk, k=k: e.activation(out=h2[:, k, :], in_=pt32[:, kk, :], func=AF.Identity,
                                                                       scale=self.modT[:, 64 + k, r:r + 1], bias=self.modT[:, 48 + k, r:r + 1]),
                             reads=[rpt32, self.Rmod, rh2], writes=[rh2])
                P.dma(S["h2T"][:, :, tt * 128:(tt + 1) * 128].rearrange("k p t -> p k t"), h2[:], reads=[rh2], writes=[RS["h2T"]])
            P.barrier()

    def layernorm(self, y, ry, st_, rst, junk, rjunk, lng, lnb, rbc, junk_n=None):
        P = self.P
        P.op("dve", lambda e: e.reduce_sum(out=st_[:, 4:5], in_=y[:], axis=AX.X), reads=[ry, rst], writes=[rst])
        if junk_n is None:
            P.op("act", lambda e: e.activation(out=junk[:], in_=y[:], func=AF.Square, accum_out=st_[:, 5:6]),
                 reads=[ry, rjunk, rst], writes=[rjunk, rst])
        else:
            npc = D // 512
            for pc_ in range(npc):
                P.op("act", lambda e, pc_=pc_: e.activation(out=junk[:, 0:512], in_=y[:, pc_ * 512:(pc_ + 1) * 512], func=AF.Square,
                                                            accum_out=st_[:, 7:8] if pc_ else st_[:, 5:6]),
                     reads=[ry, rjunk, rst], writes=[rjunk, rst])
                if pc_:
                    P.op("dve", lambda e: e.tensor_tensor(out=st_[:, 5:6], in0=st_[:, 5:6], in1=st_[:, 7:8], op=ALU.add),
                         reads=[rst], writes=[rst])
        P.op("dve", lambda e: e.tensor_scalar(out=st_[:, 4:6], in0=st_[:, 4:6], scalar1=1.0 / D, scalar2=None, op0=ALU.mult),
             reads=[rst], writes=[rst])
        P.op("dve", lambda e: e.tensor_tensor(out=st_[:, 6:7], in0=st_[:, 4:5], in1=st_[:, 4:5], op=ALU.mult), reads=[rst], writes=[rst])
        P.op("dve", lambda e: e.tensor_tensor(out=st_[:, 6:7], in0=st_[:, 5:6], in1=st_[:, 6:7], op=ALU.subtract), reads=[rst], writes=[rst])
        P.op("dve", lambda e: e.tensor_scalar(out=st_[:, 6:7], in0=st_[:, 6:7], scalar1=EPS, scalar2=None, op0=ALU.add),
             reads=[rst], writes=[rst])
        P.op("act", lambda e: e.activation(out=st_[:, 6:7], in_=st_[:, 6:7], func=AF.Ln), reads=[rst], writes=[rst])
        P.op("act", lambda e: e.activation(out=st_[:, 6:7], in_=st_[:, 6:7], func=AF.Exp, scale=-0.5), reads=[rst], writes=[rst])
        P.op("dve", lambda e: e.tensor_scalar(out=y[:], in0=y[:], scalar1=st_[:, 4:5], scalar2=st_[:, 6:7],
                                              op0=ALU.subtract, op1=ALU.mult), reads=[ry, rst], writes=[ry])
        P.op("pool", lambda e: e.tensor_tensor(out=y[:], in0=y[:], in1=lng[:], op=ALU.mult), reads=[ry, rbc], writes=[ry])
        P.op("pool", lambda e: e.tensor_tensor(out=y[:], in0=y[:], in1=lnb[:], op=ALU.add), reads=[ry, rbc], writes=[ry])

    def phase_route(self, l):
        P, C, I, S = self.P, self.C, self.I, self.S
        RS = self.RS
        tiles = list(range(NT)) if self.with_ctx else list(range(2, NT))
        with ExitStack() as ph:
            wq = self.sb(ph, "wq", [128, 16, D], BF16)
            rwq = Res("wq")
            wst = [self.sb(ph, "wst", [128, 16, 128]) for _ in range(2)]
            rws = [Res("ws0"), Res("ws1")]
            wv = I["peer_wq"][l].rearrange("(k p) n -> p k n", p=128)
            for cb in range(16):
                b = cb % 2
                P.dma(wst[b][:], wv[:, :, cb * 128:(cb + 1) * 128], writes=[rws[b]])
                P.op("pool" if cb % 2 else "dve", lambda e, b=b, cb=cb: e.tensor_copy(wq[:, :, cb * 128:(cb + 1) * 128], wst[b][:]),
                     reads=[rws[b]], writes=[rwq])
            keysT = self.sb(ph, "keysT", [128, 16, 128])
            rkT = Res("keysT")
            kst = self.sb(ph, "kst", [128, 16, 128])
            rkst = Res("kst")
            pr4 = [self.ps(ph, "prx", [128, 4, 128]) for _ in range(2)]
            rpr4 = [Res("prx0"), Res("prx1")]
            P.dma(kst[:], I["peer_keys"][l].rearrange("c k d -> k c d"), writes=[rkst])
            for cg in range(4):
                i = cg % 2
                for cc in range(4):
                    c = cg * 4 + cc
                    P.op("pe", lambda e, c=c, cc=cc: e.transpose(pr4[i][:, cc, :], kst[:, c, :], C["ident"][:]),
                         reads=[rkst, self.RC], writes=[rpr4[i]])
                P.op("dve", lambda e, cg=cg: e.tensor_copy(keysT[:, cg * 4:(cg + 1) * 4, :], pr4[i][:]), reads=[rpr4[i]], writes=[rkT])
            h2 = [self.sb(ph, "h2", [128, 16, 128], BF16) for _ in range(2)]
            rh2 = [Res("h2a"), Res("h2b")]
            qf = self.sb(ph, "qf", [128, 16, 128])
            rqf = Res("qf")
            sall = self.sb(ph, "sall", [128, 16, 128])
            rsall = Res("sall")
            wk2 = [self.sb(ph, "wk", [128, 256]) for _ in range(2)]
            rwk2 = [Res("wk0"), Res("wk1")]
            top = self.sb(ph, "top", [128, 16, 16])
            rtopc = [Res("top%d" % c) for c in range(16)]
            cand2 = [self.sb(ph, "cand", [128, 256]) for _ in range(2)]
            rcand2 = [Res("cand0"), Res("cand1")]
            best = self.sb(ph, "best", [128, 8, 16])
            rbesth = [Res("best%d" % h) for h in range(8)]
            rbest = Res("best")
            sm = self.sb(ph, "sm", [128, 4, 8])
            rsm = Res("sm")
            j16 = self.sb(ph, "j16", [128, 16])
            rj16 = Res("j16")
            s2d = [self.sb(ph, "s2d", [128, 16, 128]) for _ in range(2)]
            rs2d = [Res("s2d0"), Res("s2d1")]
            ee = [self.sb(ph, "ee", [128, 16, 128]) for _ in range(2)]
            ree = [Res("ee0"), Res("ee1")]
            gh = [self.sb(ph, "gh", [128, 16, 128], BF16) for _ in range(8)]
            rgh = [Res("gh%d" % i) for i in range(8)]
            gto = [self.sb(ph, "gto", [128, 16, 128], BF16) for _ in range(2)]
            rgto = [Res("gto0"), Res("gto1")]
            pgacc = self.ps(ph, "prg", [128, 16, 128])
            rpg = Res("prg")

            def load_t(idx):
                tt = tiles[idx]
                P.dma(h2[idx % 2][:], S["h2T"][:, :, tt * 128:(tt + 1) * 128].rearrange("k p t -> p k t"),
                      reads=[RS["h2T"]], writes=[rh2[idx % 2]])

            load_t(0)
            n = 0
            for idx, tt in enumerate(tiles):
                hb = idx % 2
                if idx + 1 < len(tiles):
                    load_t(idx + 1)
                for cg in range(4):
                    i = cg % 2
                    for cc in range(4):
                        c = cg * 4 + cc
                        for k in range(16):
                            P.op("pe", lambda e, c=c, cc=cc, k=k: e.matmul(pr4[i][:, cc, :], wq[:, k, c * 128:(c + 1) * 128], h2[hb][:, k, :],
                                                                          start=(k == 0), stop=(k == 15)),
                                 reads=[rwq, rh2[hb]], writes=[rpr4[i]])
                    P.op("act", lambda e, cg=cg: e.copy(qf[:, cg * 4:(cg + 1) * 4, :], pr4[i][:]), reads=[rpr4[i]], writes=[rqf])
                for cg in range(4):
                    i = cg % 2
                    for cc in range(4):
                        c = cg * 4 + cc
                        P.op("pe", lambda e, c=c, cc=cc: e.matmul(pr4[i][:, cc, :], qf[:, c, :], keysT[:, c, :], start=True, stop=True),
                             reads=[rqf, rkT], writes=[rpr4[i]])
                    P.op("act", lambda e, cg=cg: e.copy(sall[:, cg * 4:(cg + 1) * 4, :], pr4[i][:]), reads=[rpr4[i]], writes=[rsall])
                for c in range(16):
                    wk, rwk = wk2[c % 2], rwk2[c % 2]
                    P.op("dve", lambda e, c=c: e.max(out=top[:, c, 0:8], in_=sall[:, c, :]), reads=[rsall], writes=[rtopc[c]])
                    P.op("dve", lambda e, c=c: e.match_replace(out=wk[:, 0:128], in_to_replace=top[:, c, 0:8], in_values=sall[:, c, :],
                                                               imm_value=-1e30), reads=[rsall, rtopc[c]], writes=[rwk])
                    P.op("dve", lambda e, c=c: e.max(out=top[:, c, 8:16], in_=wk[:, 0:128]), reads=[rwk, rtopc[c]], writes=[rtopc[c]])
                for hp in range(8):
                    wk, rwk = wk2[hp % 2], rwk2[hp % 2]
                    cand, rcand = cand2[hp % 2], rcand2[hp % 2]
                    P.op("dve", lambda e, hp=hp: e.tensor_tensor(
                        out=cand[:].rearrange("p (i j) -> p i j", j=16), in0=top[:, 2 * hp, :].unsqueeze(2).to_broadcast([128, 16, 16]),
                        in1=top[:, 2 * hp + 1, :].unsqueeze(1).to_broadcast([128, 16, 16]), op=ALU.add),
                        reads=[rtopc[2 * hp], rtopc[2 * hp + 1]], writes=[rcand])
                    P.op("dve", lambda e, hp=hp: e.max(out=best[:, hp, 0:8], in_=cand[:]), reads=[rcand, rbest], writes=[rbesth[hp]])
                    P.op("dve", lambda e, hp=hp: e.match_replace(out=wk[:], in_to_replace=best[:, hp, 0:8], in_values=cand[:], imm_value=-1e30),
                         reads=[rcand, rbesth[hp]], writes=[rwk])
                    P.op("dve", lambda e, hp=hp: e.max(out=best[:, hp, 8:16], in_=wk[:]), reads=[rwk, rbesth[hp]], writes=[rbesth[hp]])
                P.op("dve", lambda e: e.tensor_scalar(out=sm[:, 0, :], in0=best[:, :, 0], scalar1=-1.0, scalar2=None, op0=ALU.mult),
                     reads=rbesth, writes=[rsm, rbest])
                for hp in range(8):
                    P.op("act", lambda e, hp=hp: e.activation(out=j16[:], in_=best[:, hp, :], func=AF.Exp, bias=sm[:, 0, hp:hp + 1],
                                                              accum_out=sm[:, 1, hp:hp + 1]), reads=[rbesth[hp], rsm, rj16], writes=[rj16, rsm])
                P.op("act", lambda e: e.activation(out=sm[:, 2, :], in_=sm[:, 1, :], func=AF.Ln), reads=[rsm], writes=[rsm])
                P.op("dve", lambda e: e.tensor_tensor(out=sm[:, 3, :], in0=sm[:, 0, :], in1=sm[:, 2, :], op=ALU.subtract),
                     reads=[rsm], writes=[rsm])
                for kb in range(8):
                    for hp in range(8):
                        i = n % 2
                        n += 1
                        P.op("pool", lambda e, hp=hp, kb=kb: e.tensor_tensor(
                            out=s2d[i][:], in0=sall[:, 2 * hp, kb * 16:(kb + 1) * 16].unsqueeze(2).to_broadcast([128, 16, 128]),
                            in1=sall[:, 2 * hp + 1, :].unsqueeze(1).to_broadcast([128, 16, 128]), op=ALU.add),
                            reads=[rsall], writes=[rs2d[i]])
                        P.op("act", lambda e, hp=hp: e.activation(out=ee[i][:], in_=s2d[i][:], func=AF.Exp, bias=sm[:, 3, hp:hp + 1]),
                             reads=[rs2d[i], rsm], writes=[ree[i]])
                        P.op("dve", lambda e, hp=hp: e.scalar_tensor_tensor(out=gh[hp][:], in0=s2d[i][:], scalar=best[:, hp, 15:16],
                                                                           in1=ee[i][:], op0=ALU.is_ge, op1=ALU.mult),
                             reads=[rs2d[i], ree[i], rbesth[hp]], writes=[rgh[hp]])
                    for kk in range(16):
                        for hp in range(8):
                            P.op("pe", lambda e, kk=kk, hp=hp: e.matmul(pgacc[:, kk, :], gh[hp][:, kk, :], C["identb"][:],
                                                                        start=(hp == 0), stop=(hp == 7)),
                                 reads=[rgh[hp], self.RC], writes=[rpg])
                    o_ = kb % 2
                    P.op("act", lambda e: e.copy(gto[o_][:], pgacc[:]), reads=[rpg], writes=[rgto[o_]])
                    P.dma(S["gT"][kb * 16:(kb + 1) * 16, :, tt * 128:(tt + 1) * 128].rearrange("a k t -> k a t"), gto[o_][:],
                          reads=[rgto[o_]], writes=[RS["gT"]])
            P.barrier()

    def phase_peer(self, l):
        P, C, I, S = self.P, self.C, self.I, self.S
        RS = self.RS
        tiles = list(range(NT)) if self.with_ctx else list(range(2, NT))
        GT = 9 if len(tiles) == 18 else 8
        groups = [tiles[i:i + GT] for i in range(0, len(tiles), GT)]
        GW = GT * 128
        SC = 4
        with ExitStack() as ph:
            acc = self.sb(ph, "oacc", [128, GT, D])
            racc = Res("oacc")
            raccj = [[Res("oacc%d_%d" % (j, hf)) for hf in range(2)] for j in range(GT)]
            h2g = self.sb(ph, "h2g", [128, 16, GW], BF16)
            rh2g = Res("h2g")
            ust2 = [self.sb(ph, "ust", [128, D]) for _ in range(2)]
            vst2 = [self.sb(ph, "vst", [128, D]) for _ in range(2)]
            ub = self.sb(ph, "ub", [128, D], BF16)
            rust2 = [Res("ust0"), Res("ust1")]
            rvst2 = [Res("vst0"), Res("vst1")]
            rub = Res("ub")
            uT = [self.sb(ph, "uT", [128, 16, 128], BF16) for _ in range(SC)]
            vb = [self.sb(ph, "vb", [128, D], BF16) for _ in range(SC)]
            gt = [self.sb(ph, "gt", [128, GW], BF16) for _ in range(SC)]
            wt = [self.sb(ph, "wt", [128, GW], BF16) for _ in range(SC)]
            ruT = [Res("uT%d" % i) for i in range(SC)]
            rvb = [Res("vb%d" % i) for i in range(SC)]
            rgt = [Res("gt%d" % i) for i in range(SC)]
            rwt = [Res("wt%d" % i) for i in range(SC)]
            act = self.sb(ph, "act", [128, GW])
            ract = Res("act")
            ptp2 = [self.ps(ph, "ptp", [128, 4, 128], BF16) for _ in range(2)]
            rptp2 = [Res("ptp"), Res("ptp")]
            rptp2[1] = Res("ptp")
            pa = [self.ps(ph, "pa", [128, 512]) for _ in range(2)]
            rpa = [Res("pa0"), Res("pa1")]
            pmh = [self.ps(ph, "pm", [128, 1024]) for _ in range(2)]
            rpmh = [Res("pm0"), Res("pm1")]
            ntp = 0
            na = 0
            for grp in groups:
                ng = len(grp)
                nt_ = ng * 128
                t0 = grp[0] * 128
                P.dma(h2g[:, :, 0:nt_], S["h2T"][:, :, t0:t0 + nt_].rearrange("k p t -> p k t"), reads=[RS["h2T"]], writes=[rh2g])
                P.op("pool", lambda e: e.memset(acc[:], 0.0), reads=[racc] + [r_ for rr in raccj for r_ in rr],
                     writes=[racc] + [r_ for rr in raccj for r_ in rr])
                for sc0 in range(0, 128, SC):
                    for si in range(SC):
                        ec = sc0 + si
                        ust, vst, rust, rvst = ust2[ec % 2], vst2[ec % 2], rust2[ec % 2], rvst2[ec % 2]
                        P.dma(ust[:], I["peer_u"][l, ec * 128:(ec + 1) * 128, :], writes=[rust])
                        P.dma(vst[:], I["peer_v"][l, ec * 128:(ec + 1) * 128, :], writes=[rvst])
                        P.dma(gt[si][:, 0:nt_], S["gT"][ec, :, t0:t0 + nt_], reads=[RS["gT"]], writes=[rgt[si]])
                        P.op("dve", lambda e: e.tensor_copy(ub[:], ust[:]), reads=[rust], writes=[rub])
                        P.op("pool", lambda e, si=si: e.tensor_copy(vb[si][:], vst[:]), reads=[rvst], writes=[rvb[si]])
                        for kg in range(4):
                            ptp, rptp = ptp2[ntp % 2], rptp2[ntp % 2]
                            ntp += 1
                            for kk in range(4):
                                k = kg * 4 + kk
                                P.op("pe", lambda e, kk=kk, k=k: e.transpose(ptp[:, kk, :], ub[:, k * 128:(k + 1) * 128], C["identb"][:]),
                                     reads=[rub, self.RC], writes=[rptp])
                            P.op("act", lambda e, kg=kg, si=si: e.copy(uT[si][:, kg * 4:(kg + 1) * 4, :], ptp[:]),
                                 reads=[rptp], writes=[ruT[si]])
                        for tb0 in range(0, nt_, 512):
                            nb_ = min(512, nt_ - tb0)
                            i = na % 2
                            na += 1
                            for k in range(16):
                                P.op("pe", lambda e, k=k, si=si: e.matmul(pa[i][:, 0:nb_], uT[si][:, k, :], h2g[:, k, tb0:tb0 + nb_],
                                                                          start=(k == 0), stop=(k == 15)),
                                     reads=[ruT[si], rh2g], writes=[rpa[i]])
                            P.op("act", lambda e: e.activation(out=act[:, tb0:tb0 + nb_], in_=pa[i][:, 0:nb_], func=AF.Gelu),
                                 reads=[rpa[i]], writes=[ract])
                        P.op("dve", lambda e, si=si: e.tensor_tensor(out=wt[si][:, 0:nt_], in0=act[:, 0:nt_], in1=gt[si][:, 0:nt_], op=ALU.mult),
                             reads=[ract, rgt[si]], writes=[rwt[si]])
                    for j in range(ng):
                        for hf in range(2):
                            for n2 in range(2):
                                nb4 = hf * 2 + n2
                                for si in range(SC):
                                    P.op("pe", lambda e, j=j, nb4=nb4, n2=n2, hf=hf, si=si: e.matmul(
                                        pmh[hf][:, n2 * 512:(n2 + 1) * 512], wt[si][:, j * 128:(j + 1) * 128],
                                        vb[si][:, nb4 * 512:(nb4 + 1) * 512],
                                        start=(si == 0), stop=(si == SC - 1)), reads=[rwt[si], rvb[si]], writes=[rpmh[hf]])
                            P.op("dve", lambda e, j=j, hf=hf: e.tensor_tensor(out=acc[:, j, hf * 1024:(hf + 1) * 1024],
                                                                              in0=acc[:, j, hf * 1024:(hf + 1) * 1024], in1=pmh[hf][:], op=ALU.add),
                                 reads=[raccj[j][hf], rpmh[hf]], writes=[raccj[j][hf]])
                for j, tt in enumerate(grp):
                    P.dma(S["fD"][tt * 128:(tt + 1) * 128, :], acc[:, j, :], reads=[racc] + raccj[j], writes=[RS["fD"]])
            P.barrier()
        with ExitStack() as ph:
            gbc = [self.sb(ph, "gbc", [128, D]) for _ in range(2)]
            lng = self.sb(ph, "lng", [128, D])
            lnb = self.sb(ph, "lnb", [128, D])
            rbc = Res("bcast")
            P.dma(gbc[0][:], S["gv"][2:3, :].partition_broadcast(128), reads=[RS["gv"]], writes=[rbc])
            P.dma(gbc[1][:], S["gv"][3:4, :].partition_broadcast(128), reads=[RS["gv"]], writes=[rbc])
            P.dma(lng[:], I["ln2_g"][l:l + 1, :].partition_broadcast(128), writes=[rbc])
            P.dma(lnb[:], I["ln2_b"][l:l + 1, :].partition_broadcast(128), writes=[rbc])
            xin = [self.sb(ph, "xin", [128, D]) for _ in range(2)]
            fin = [self.sb(ph, "fin", [128, D]) for _ in range(2)]
            rxin = [Res("xin0"), Res("xin1")]
            rfin = [Res("fin0"), Res("fin1")]
            y2 = [self.sb(ph, "y", [128, D]) for _ in range(2)]
            ry2 = [Res("y0"), Res("y1")]
            st2 = [self.sb(ph, "st", [128, 8]) for _ in range(2)]
            rst2 = [Res("st0"), Res("st1")]

            def load_ln(idx):
                tt = tiles[idx]
                b = idx % 2
                P.dma(xin[b][:], S["xres"][tt * 128:(tt + 1) * 128, :], reads=[RS["xres"]], writes=[rxin[b]])
                P.dma(fin[b][:], S["fD"][tt * 128:(tt + 1) * 128, :], reads=[RS["fD"]], writes=[rfin[b]])

            load_ln(0)
            for idx, tt in enumerate(tiles):
                b = idx % 2
                r = 1 if tt < 2 else 0
                if idx + 1 < len(tiles):
                    load_ln(idx + 1)
                y, ry = y2[b], ry2[b]
                P.op("dve", lambda e: e.tensor_tensor(out=y[:], in0=fin[b][:], in1=gbc[r][:], op=ALU.mult),
                     reads=[rfin[b], rbc, ry], writes=[ry])
                P.op("dve", lambda e: e.scalar_tensor_tensor(out=y[:], in0=xin[b][:], scalar=ALPHA, in1=y[:], op0=ALU.mult, op1=ALU.add),
                     reads=[rxin[b], ry], writes=[ry])
                self.layernorm(y, ry, st2[b], rst2[b], fin[b], rfin[b], lng, lnb, rbc)
                P.dma(S["xres"][tt * 128:(tt + 1) * 128, :], y[:], reads=[ry], writes=[RS["xres"]])
            P.barrier()


def make_inputs(inputs, depth=DEPTH):
    f = lambda a: np.ascontiguousarray(np.asarray(a, dtype=np.float32))
    cst = host_consts()
    shared = {
        "ada_w": f(inputs["ada_w"][:depth]),
        "ada_b": f(inputs["ada_b"][:depth]).reshape(depth, 96, 128),
        "w_in": f(inputs["w_in"][:depth]),
        "conv_w": f(inputs["conv_w"][:depth]).reshape(depth, 5, 12, 128).reshape(depth, 60, 128),
        "conv_b": f(inputs["conv_b"][:depth]).reshape(depth, 12, 128),
        "a_log": f(inputs["a_log"][:depth]).reshape(depth, 32),
        "dt_bias": f(inputs["dt_bias"][:depth]).reshape(depth, 32),
        "d_skip": f(inputs["d_skip"][:depth]).reshape(depth, 32),
        "biasT": host_bias(f(inputs["rpb"][:depth])),
        "beta": np.concatenate([f(inputs["beta_attn"][:depth]), f(inputs["beta_ssm"][:depth])], axis=1),
        "w_out": f(inputs["w_out"][:depth]),
        "ln1_g": f(inputs["ln1_g"][:depth]), "ln1_b": f(inputs["ln1_b"][:depth]),
        "ln2_g": f(inputs["ln2_g"][:depth]), "ln2_b": f(inputs["ln2_b"][:depth]),
        "peer_wq": f(inputs["peer_wq"][:depth]),
        "peer_keys": f(inputs["peer_keys"][:depth]).reshape(depth, 16, 128, 128),
        "peer_u": f(inputs["peer_u"][:depth]),
        "peer_v": f(inputs["peer_v"][:depth]),
    }
    shared.update(cst)
    return shared


def kernel(**inputs):
    shared = make_inputs(inputs)
    B = inputs["x"].shape[0]
    nc = Builder().build()
    in_maps = []
    for b in range(B):
        m = dict(shared)
        m["x"] = np.ascontiguousarray(inputs["x"][b], dtype=np.float32)
        m["ctx"] = np.ascontiguousarray(inputs["ctx"][b], dtype=np.float32)
        m["c2"] = np.concatenate([np.asarray(inputs["c"][b], np.float32).reshape(16, 128),
                                  np.asarray(inputs["c_ctx"], np.float32).reshape(16, 128)], axis=0)
        in_maps.append(m)
    res = run_bass_kernel_spmd(nc, in_maps, core_ids=list(range(B)))
    return np.stack([np.asarray(r["out"], dtype=np.float32) for r in res.results], axis=0)
```
